# Optimizing a Trainium2 kernel written in Bass

```python
import math
import jax, jax.numpy as jnp
from jax import lax
import numpy as np

D_MODEL = 1024
BATCH = 16
SEQ = 2048
DEPTH = 2

MEM_LEN = 256
ATTN_HEADS = 8
ATTN_HEAD_DIM = 64
D_ATTN = ATTN_HEADS * ATTN_HEAD_DIM
D_SSM = D_MODEL // 4
SSM_GROUP = 16
SSM_GROUPS = D_SSM // SSM_GROUP
SSM_STATE = 64
D_LRU = D_MODEL // 4
LRU_HEADS = 4
LRU_HEAD_DIM = D_LRU // LRU_HEADS
CONV_WIDTH = 4
LRU_C = 8.0
D_MIX = D_ATTN + D_SSM + D_LRU
D_IN = 3 * D_ATTN + D_SSM + 2 * D_LRU
SPLITS = (D_ATTN, 2 * D_ATTN, 3 * D_ATTN, 3 * D_ATTN + D_SSM, 3 * D_ATTN + D_SSM + D_LRU)
MOBA_BLOCK = 256
MOBA_TOPK = 3
MOBA_Q_CHUNK = 16
MEM_HEADS = 4
MEM_HEAD_DIM = D_MODEL // MEM_HEADS
N_GROUPS = 4
EXPERTS_PER_GROUP = 4
N_EXPERTS = N_GROUPS * EXPERTS_PER_GROUP
EXPERT_TOPK = 2
D_EXPERT = 256
ALPHA = (2.0 * DEPTH) ** 0.25
BETA = (8.0 * DEPTH) ** -0.25
LN_EPS = 1e-5
RMS_EPS = 1e-6
NEG_INF = -1e30

kernel_name = "hymba_moba_s5_rglru_hmoe_deepnorm"


def layer_norm(x, g, b):
    xf = x.astype(jnp.float32)
    mu = jnp.mean(xf, axis=-1, keepdims=True)
    var = jnp.mean(jnp.square(xf - mu), axis=-1, keepdims=True)
    y = (xf - mu) * lax.rsqrt(var + LN_EPS) * g.astype(jnp.float32) + b.astype(jnp.float32)
    return y.astype(x.dtype)


def rms_normalize(x):
    xf = x.astype(jnp.float32)
    return (xf * lax.rsqrt(jnp.mean(xf * xf, axis=-1, keepdims=True) + RMS_EPS)).astype(x.dtype)


def alibi_slopes(n_heads):
    return jnp.asarray([2.0 ** (-8.0 * (h + 1) / n_heads) for h in range(n_heads)], jnp.float32)


def moba_attention(q, k, v):
    bsz, s, nh, dh = q.shape
    f32 = jnp.float32
    nb = -(-s // MOBA_BLOCK)
    sp = nb * MOBA_BLOCK
    kk = min(MOBA_TOPK, nb)
    nc = sp // MOBA_Q_CHUNK
    pad = ((0, 0), (0, sp - s), (0, 0), (0, 0))
    q, k, v = [jnp.pad(t, pad).transpose(0, 2, 1, 3) for t in (q, k, v)]
    kb = k.reshape(bsz, nh, nb, MOBA_BLOCK, dh)
    vb = v.reshape(bsz, nh, nb, MOBA_BLOCK, dh)
    kmean = jnp.mean(kb.astype(f32), axis=3)
    gate = jnp.einsum('bhtd,bhnd->bhtn', q.astype(f32), kmean)
    qblk = jnp.arange(sp) // MOBA_BLOCK
    fully_past = jnp.arange(nb)[None, :] < qblk[:, None]
    gate = jnp.where(fully_past, gate, NEG_INF)
    _, sel = lax.top_k(gate, kk)
    slopes = alibi_slopes(nh)
    scale = dh ** -0.5
    q_chunks = q.reshape(bsz, nh, nc, MOBA_Q_CHUNK, dh).transpose(2, 0, 1, 3, 4)
    sel_chunks = sel.reshape(bsz, nh, nc, MOBA_Q_CHUNK, kk).transpose(2, 0, 1, 3, 4)
    gather_blocks = jax.vmap(jax.vmap(lambda blocks, idx: blocks[idx]))
    offs = jnp.arange(MOBA_BLOCK)

    def attend_chunk(args):
        c, q_c, sel_c = args
        t = c * MOBA_Q_CHUNK + jnp.arange(MOBA_Q_CHUNK)
        own = (c * MOBA_Q_CHUNK) // MOBA_BLOCK
        k_sel = gather_blocks(kb, sel_c)
        v_sel = gather_blocks(vb, sel_c)
        s_sel = jnp.einsum('bhqd,bhqjsd->bhqjs', q_c, k_sel).astype(f32) * scale
        dist_sel = (t[:, None, None] - (sel_c[..., None] * MOBA_BLOCK + offs)).astype(f32)
        s_sel = s_sel - slopes[None, :, None, None, None] * dist_sel
        sel_ok = jnp.arange(kk) < own
        s_sel = jnp.where(sel_ok[:, None], s_sel, NEG_INF)
        k_own = lax.dynamic_index_in_dim(kb, own, axis=2, keepdims=False)
        v_own = lax.dynamic_index_in_dim(vb, own, axis=2, keepdims=False)
        s_own = jnp.einsum('bhqd,bhsd->bhqs', q_c, k_own).astype(f32) * scale
        dist_own = (t[:, None] - (own * MOBA_BLOCK + offs)[None, :]).astype(f32)
        s_own = jnp.where(dist_own >= 0, s_own - slopes[:, None, None] * dist_own, NEG_INF)
        scores = jnp.concatenate([s_sel.reshape(bsz, nh, MOBA_Q_CHUNK, kk * MOBA_BLOCK), s_own], axis=-1)
        p = jax.nn.softmax(scores, axis=-1).astype(v_sel.dtype)
        p_sel = p[..., :kk * MOBA_BLOCK].reshape(bsz, nh, MOBA_Q_CHUNK, kk, MOBA_BLOCK)
        p_own = p[..., kk * MOBA_BLOCK:]
        return (jnp.einsum('bhqjs,bhqjsd->bhqd', p_sel, v_sel)
                + jnp.einsum('bhqs,bhsd->bhqd', p_own, v_own))

    out = lax.map(attend_chunk, (jnp.arange(nc), q_chunks, sel_chunks))
    out = out.transpose(1, 0, 3, 2, 4).reshape(bsz, sp, nh * dh)
    return out[:, :s]


def s5_mixer(u, a_re, a_im, b_re, b_im, c_re, c_im, d_skip, log_dt, w_glu, b_glu):
    f32 = jnp.float32
    bsz, s, _ = u.shape
    uf = u.astype(f32).reshape(bsz, s, SSM_GROUPS, SSM_GROUP)
    dt = jnp.exp(log_dt.astype(f32))[:, None]
    are, aim = a_re.astype(f32), a_im.astype(f32)
    mag = jnp.exp(dt * are)
    ab_re, ab_im = mag * jnp.cos(dt * aim), mag * jnp.sin(dt * aim)
    den = are * are + aim * aim
    nr, ni = ab_re - 1.0, ab_im
    f_re = (nr * are + ni * aim) / den
    f_im = (ni * are - nr * aim) / den
    br, bi = b_re.astype(f32), b_im.astype(f32)
    bb_re = f_re[..., None] * br - f_im[..., None] * bi
    bb_im = f_re[..., None] * bi + f_im[..., None] * br
    bu_re = jnp.einsum('gph,bsgh->bsgp', bb_re, uf)
    bu_im = jnp.einsum('gph,bsgh->bsgp', bb_im, uf)
    at_re = jnp.broadcast_to(ab_re, bu_re.shape)
    at_im = jnp.broadcast_to(ab_im, bu_im.shape)

    def combine(e1, e2):
        a1r, a1i, b1r, b1i = e1
        a2r, a2i, b2r, b2i = e2
        return (a2r * a1r - a2i * a1i, a2r * a1i + a2i * a1r,
                a2r * b1r - a2i * b1i + b2r, a2r * b1i + a2i * b1r + b2i)

    _, _, xr, xi = lax.associative_scan(combine, (at_re, at_im, bu_re, bu_im), axis=1)
    y = (jnp.einsum('ghp,bsgp->bsgh', c_re.astype(f32), xr)
         - jnp.einsum('ghp,bsgp->bsgh', c_im.astype(f32), xi)).reshape(bsz, s, D_SSM)
    y = jax.nn.gelu(y + d_skip.astype(f32) * u.astype(f32))
    y = y * jax.nn.sigmoid(y @ w_glu.astype(f32) + b_glu.astype(f32))
    return y.astype(u.dtype)


def rglru_mixer(xl, gl, conv_w, conv_b, w_a, b_a, w_x, b_x, lam):
    f32 = jnp.float32
    bsz, s, _ = xl.shape
    xc = lax.conv_general_dilated(xl.astype(f32), conv_w.astype(f32)[:, None, :], window_strides=(1,),
                                  padding=[(CONV_WIDTH - 1, 0)], dimension_numbers=('NWC', 'WIO', 'NWC'),
                                  feature_group_count=D_LRU) + conv_b.astype(f32)
    xh = xc.reshape(bsz, s, LRU_HEADS, LRU_HEAD_DIM)
    r = jax.nn.sigmoid(jnp.einsum('bshi,hij->bshj', xh, w_a.astype(f32)).reshape(bsz, s, D_LRU) + b_a.astype(f32))
    i = jax.nn.sigmoid(jnp.einsum('bshi,hij->bshj', xh, w_x.astype(f32)).reshape(bsz, s, D_LRU) + b_x.astype(f32))
    log_a = -LRU_C * r * jax.nn.softplus(-lam.astype(f32))
    a = jnp.exp(log_a)
    bterm = jnp.sqrt(-jnp.expm1(2.0 * log_a)) * (i * xc)
    _, hseq = lax.associative_scan(lambda e1, e2: (e1[0] * e2[0], e2[0] * e1[1] + e2[1]), (a, bterm), axis=1)
    return (hseq * jax.nn.gelu(gl.astype(f32))).astype(xl.dtype)


def hybrid_mixer(h, w_in, mix_g, w_out, a_re, a_im, b_re, b_im, c_re, c_im, d_skip, log_dt, w_glu, b_glu,
                 conv_w, conv_b, w_a, b_a, w_x, b_x, lam):
    bsz, s, _ = h.shape
    z = h @ w_in
    q, k, v, u, xl, gl = jnp.split(z, SPLITS, axis=-1)
    hd = (bsz, s, ATTN_HEADS, ATTN_HEAD_DIM)
    y_attn = moba_attention(q.reshape(hd), k.reshape(hd), v.reshape(hd))
    y_ssm = s5_mixer(u, a_re, a_im, b_re, b_im, c_re, c_im, d_skip, log_dt, w_glu, b_glu)
    y_lru = rglru_mixer(xl, gl, conv_w, conv_b, w_a, b_a, w_x, b_x, lam)
    y = jnp.concatenate([rms_normalize(y_attn), rms_normalize(y_ssm), rms_normalize(y_lru)], axis=-1)
    return (y * mix_g) @ w_out


def memory_cross_attention(h, mem, wq, wk, wv, wo):
    bsz, s, _ = h.shape
    m = mem.shape[1]
    q = (h @ wq).reshape(bsz, s, MEM_HEADS, MEM_HEAD_DIM)
    k = (mem @ wk).reshape(bsz, m, MEM_HEADS, MEM_HEAD_DIM)
    v = (mem @ wv).reshape(bsz, m, MEM_HEADS, MEM_HEAD_DIM)
    sc = jnp.einsum('bqhd,bkhd->bhqk', q, k).astype(jnp.float32) * (MEM_HEAD_DIM ** -0.5)
    p = jax.nn.softmax(sc, axis=-1).astype(v.dtype)
    o = jnp.einsum('bhqk,bkhd->bqhd', p, v).reshape(bsz, s, D_MODEL)
    return o @ wo


def hier_moe(h, wr_g, br_g, wr_e, br_e, w_gate, w_up, w_down):
    f32 = jnp.float32
    bsz, s, d = h.shape
    t = h.reshape(bsz * s, d)
    g_logits = (t @ wr_g).astype(f32) + br_g.astype(f32)
    g_prob = jax.nn.softmax(g_logits, axis=-1)
    g_onehot = jax.nn.one_hot(jnp.argmax(g_logits, axis=-1), N_GROUPS, dtype=f32)
    g_w = jnp.sum(g_prob * g_onehot, axis=-1, keepdims=True)
    e_logits = ((t @ wr_e).astype(f32) + br_e.astype(f32)).reshape(-1, N_GROUPS, EXPERTS_PER_GROUP)
    e_in_group = jnp.einsum('ng,nge->ne', g_onehot, e_logits)
    e_prob = jax.nn.softmax(e_in_group, axis=-1)
    top_p, top_i = lax.top_k(e_prob, EXPERT_TOPK)
    top_p = top_p / jnp.sum(top_p, axis=-1, keepdims=True)
    local = jnp.sum(jax.nn.one_hot(top_i, EXPERTS_PER_GROUP, dtype=f32) * top_p[..., None], axis=1)
    combine = (g_onehot[:, :, None] * local[:, None, :] * g_w[:, :, None]).reshape(-1, N_EXPERTS).astype(t.dtype)
    out = jnp.zeros_like(t)
    for e in range(N_EXPERTS):
        he = jax.nn.silu(t @ w_gate[e]) * (t @ w_up[e])
        out = out + combine[:, e:e + 1] * (he @ w_down[e])
    return out.reshape(bsz, s, d)


def setup_inputs(seed: int = 0) -> dict:
    key = jax.random.key(seed)
    keys = jax.random.split(key, 64)
    counter = [0]

    def nk():
        kk_ = keys[counter[0]]
        counter[0] += 1
        return kk_

    def nrm(shape, scale):
        return scale * jax.random.normal(nk(), shape, jnp.float32)

    def gain(shape):
        return 1.0 + nrm(shape, 0.02)

    L, d = DEPTH, D_MODEL
    G, P, H = SSM_GROUPS, SSM_STATE, SSM_GROUP
    inp = {}
    inp['x'] = nrm((BATCH, SEQ, d), 1.0)
    inp['mem'] = nrm((BATCH, MEM_LEN, d), 1.0)
    inp['ln0_g'] = gain((d,))
    inp['ln0_b'] = nrm((d,), 0.02)
    inp['w_in'] = nrm((L, d, D_IN), d ** -0.5)
    inp['mix_g'] = gain((L, D_MIX))
    inp['w_out'] = nrm((L, D_MIX, d), BETA * D_MIX ** -0.5)
    inp['ssm_a_re'] = -0.5 + nrm((L, G, P), 0.01)
    inp['ssm_a_im'] = math.pi * jnp.arange(P, dtype=jnp.float32) + nrm((L, G, P), 0.01)
    inp['ssm_b_re'] = nrm((L, G, P, H), (2.0 * H) ** -0.5)
    inp['ssm_b_im'] = nrm((L, G, P, H), (2.0 * H) ** -0.5)
    inp['ssm_c_re'] = nrm((L, G, H, P), P ** -0.5)
    inp['ssm_c_im'] = nrm((L, G, H, P), P ** -0.5)
    inp['ssm_d'] = nrm((L, D_SSM), 1.0)
    inp['ssm_log_dt'] = jax.random.uniform(nk(), (L, G), jnp.float32, math.log(1e-3), math.log(1e-1))
    inp['ssm_w_glu'] = nrm((L, D_SSM, D_SSM), D_SSM ** -0.5)
    inp['ssm_b_glu'] = nrm((L, D_SSM), 0.01)
    inp['lru_conv_w'] = nrm((L, CONV_WIDTH, D_LRU), CONV_WIDTH ** -0.5)
    inp['lru_conv_b'] = nrm((L, D_LRU), 0.01)
    inp['lru_w_a'] = nrm((L, LRU_HEADS, LRU_HEAD_DIM, LRU_HEAD_DIM), LRU_HEAD_DIM ** -0.5)
    inp['lru_b_a'] = nrm((L, D_LRU), 0.01)
    inp['lru_w_x'] = nrm((L, LRU_HEADS, LRU_HEAD_DIM, LRU_HEAD_DIM), LRU_HEAD_DIM ** -0.5)
    inp['lru_b_x'] = nrm((L, D_LRU), 0.01)
    a_pow_c = jax.random.uniform(nk(), (L, D_LRU), jnp.float32, 0.9, 0.999)
    a_base = a_pow_c ** (1.0 / LRU_C)
    inp['lru_lam'] = jnp.log(a_base) - jnp.log1p(-a_base)
    inp['ln1_g'] = gain((L, d))
    inp['ln1_b'] = nrm((L, d), 0.02)
    inp['mem_wq'] = nrm((L, d, d), d ** -0.5)
    inp['mem_wk'] = nrm((L, d, d), d ** -0.5)
    inp['mem_wv'] = nrm((L, d, d), d ** -0.5)
    inp['mem_wo'] = nrm((L, d, d), BETA * d ** -0.5)
    inp['ln2_g'] = gain((L, d))
    inp['ln2_b'] = nrm((L, d), 0.02)
    inp['moe_wr_g'] = nrm((L, d, N_GROUPS), d ** -0.5)
    inp['moe_br_g'] = nrm((L, N_GROUPS), 0.01)
    inp['moe_wr_e'] = nrm((L, d, N_EXPERTS), d ** -0.5)
    inp['moe_br_e'] = nrm((L, N_EXPERTS), 0.01)
    inp['moe_w_gate'] = nrm((L, N_EXPERTS, d, D_EXPERT), d ** -0.5)
    inp['moe_w_up'] = nrm((L, N_EXPERTS, d, D_EXPERT), d ** -0.5)
    inp['moe_w_down'] = nrm((L, N_EXPERTS, D_EXPERT, d), BETA * D_EXPERT ** -0.5)
    inp['ln3_g'] = gain((L, d))
    inp['ln3_b'] = nrm((L, d), 0.02)
    return inp


def reference(x, mem, ln0_g, ln0_b, w_in, mix_g, w_out,
              ssm_a_re, ssm_a_im, ssm_b_re, ssm_b_im, ssm_c_re, ssm_c_im, ssm_d, ssm_log_dt, ssm_w_glu, ssm_b_glu,
              lru_conv_w, lru_conv_b, lru_w_a, lru_b_a, lru_w_x, lru_b_x, lru_lam,
              ln1_g, ln1_b, mem_wq, mem_wk, mem_wv, mem_wo, ln2_g, ln2_b,
              moe_wr_g, moe_br_g, moe_wr_e, moe_br_e, moe_w_gate, moe_w_up, moe_w_down, ln3_g, ln3_b):
    h = layer_norm(x, ln0_g, ln0_b)
    for l in range(DEPTH):
        mix = hybrid_mixer(h, w_in[l], mix_g[l], w_out[l],
                           ssm_a_re[l], ssm_a_im[l], ssm_b_re[l], ssm_b_im[l], ssm_c_re[l], ssm_c_im[l],
                           ssm_d[l], ssm_log_dt[l], ssm_w_glu[l], ssm_b_glu[l],
                           lru_conv_w[l], lru_conv_b[l], lru_w_a[l], lru_b_a[l], lru_w_x[l], lru_b_x[l], lru_lam[l])
        h = layer_norm(ALPHA * h + mix, ln1_g[l], ln1_b[l])
        cross = memory_cross_attention(h, mem, mem_wq[l], mem_wk[l], mem_wv[l], mem_wo[l])
        h = layer_norm(ALPHA * h + cross, ln2_g[l], ln2_b[l])
        ffn = hier_moe(h, moe_wr_g[l], moe_br_g[l], moe_wr_e[l], moe_br_e[l],
                       moe_w_gate[l], moe_w_up[l], moe_w_down[l])
        h = layer_norm(ALPHA * h + ffn, ln3_g[l], ln3_b[l])
    return h
```

```python
import math
from contextlib import ExitStack
import numpy as np
import ml_dtypes
import concourse.bass as bass
import concourse.mybir as mybir
from concourse.bass_utils import run_bass_kernel_spmd

F32 = mybir.dt.float32
BF16 = mybir.dt.bfloat16
I32 = mybir.dt.int32
AF = mybir.ActivationFunctionType
ALU = mybir.AluOpType
AX = mybir.AxisListType

DEBUG = False
ARENA_LOG = None
STOP_AFTER = None

NCORES = 8
S = 2048
NTOK = 2 * S
D = 1024
ALPHA = (2.0 * 2) ** 0.25
LN_EPS = 1e-5
RMS_EPS = 1e-6
NEGM = -30000.0
TWO_PI = 2.0 * math.pi


class Prog:
    ENGS = ("pe", "act", "dve", "pool", "sp")
    NDMA = 24

    def __init__(self, nc):
        self.nc = nc
        self.ins = []
        self.lastw = {}
        self.readers = {}
        self.bar = None
        self._bar_start = 0

    def _add(self, eng, fn, reads, writes, dma):
        idx = len(self.ins)
        deps = set()
        if self.bar is not None:
            deps.update(self.bar)
        for r in reads:
            w = self.lastw.get(r)
            if w is not None:
                deps.add(w)
        for w_ in writes:
            w = self.lastw.get(w_)
            if w is not None:
                deps.add(w)
            for rd in self.readers.get(w_, ()):
                deps.add(rd)
        deps.discard(idx)
        self.ins.append(dict(eng=eng, fn=fn, deps=deps, dma=dma, idx=idx))
        for r in reads:
            self.readers.setdefault(r, []).append(idx)
        for w_ in writes:
            self.lastw[w_] = idx
            self.readers[w_] = []
        return idx

    def op(self, eng, fn, reads=(), writes=()):
        return self._add(eng, fn, tuple(reads), tuple(writes), False)

    def dma(self, eng, fn, reads=(), writes=()):
        return self._add(eng, fn, tuple(reads), tuple(writes), True)

    def barrier(self):
        last = {}
        dmas = []
        for i in self.ins[self._bar_start:]:
            if i["dma"]:
                dmas.append(i["idx"])
            else:
                last[i["eng"]] = i["idx"]
        nb = set(last.values()) | set(dmas)
        if self.bar is not None:
            engs_new = {self.ins[j]["eng"] for j in last.values()}
            for j in self.bar:
                if self.ins[j]["dma"] or self.ins[j]["eng"] not in engs_new:
                    if not self.ins[j]["dma"]:
                        nb.add(j)
        self.bar = nb
        self._bar_start = len(self.ins)
        self.lastw = {}
        self.readers = {}

    def emit(self, stack):
        nc = self.nc
        ins = self.ins
        for i in ins:
            nd = set()
            for d in i["deps"]:
                p = ins[d]
                if (not p["dma"]) and (not i["dma"]) and p["eng"] == "pe" and i["eng"] == "pe":
                    continue
                nd.add(d)
            i["deps"] = nd
        needed = set()
        for i in ins:
            needed.update(i["deps"])
        esem = {e: stack.enter_context(nc.semaphore("s_" + e)) for e in self.ENGS}
        dsem = [stack.enter_context(nc.semaphore("d_%d" % k)) for k in range(self.NDMA)]
        cnt = {e: 0 for e in self.ENGS}
        dcnt = [0] * self.NDMA
        nd_ = 0
        for i in ins:
            if i["dma"]:
                k = nd_ % self.NDMA
                nd_ += 1
                i["prev"] = (dsem[k], dcnt[k]) if dcnt[k] > 0 else None
                dcnt[k] += 16
                i["sem"] = dsem[k]
                i["val"] = dcnt[k]
                i["semkey"] = ("d", k)
                i["signal"] = True
            else:
                if i["idx"] in needed:
                    cnt[i["eng"]] += 1
                    i["signal"] = True
                else:
                    i["signal"] = False
                i["sem"] = esem[i["eng"]]
                i["val"] = cnt[i["eng"]]
                i["semkey"] = ("e", i["eng"])
        per = {e: [i for i in ins if i["eng"] == e] for e in self.ENGS}
        block = stack.enter_context(nc.Block())

        def run(eng_obj, lst):
            waited = {}
            for i in lst:
                waits = {}
                for d in i["deps"]:
                    p = ins[d]
                    k = p["semkey"]
                    if waits.get(k, (None, 0))[1] < p["val"]:
                        waits[k] = (p["sem"], p["val"])
                if i["dma"] and i["prev"] is not None:
                    k = i["semkey"]
                    if waits.get(k, (None, 0))[1] < i["prev"][1]:
                        waits[k] = i["prev"]
                for k, (s, v) in waits.items():
                    if waited.get(k, 0) >= v:
                        continue
                    eng_obj.wait_ge(s, v)
                    waited[k] = v
                r = i["fn"](eng_obj)
                if i["dma"]:
                    r.then_inc(i["sem"], 16)
                elif i["signal"]:
                    r.then_inc(i["sem"], 1)
            return waited

        fin = {}
        for i in ins:
            if i["dma"]:
                fin[i["semkey"]] = (i["sem"], i["val"])

        @block.tensor
        def _(e):
            run(e, per["pe"])

        @block.scalar
        def _(e):
            run(e, per["act"])

        @block.vector
        def _(e):
            run(e, per["dve"])

        @block.gpsimd
        def _(e):
            waited = run(e, per["pool"])

        @block.sync
        def _(e):
            waited = run(e, per["sp"])
            for k, (s, v) in fin.items():
                if waited.get(k, 0) < v:
                    e.wait_ge(s, v)


def resh(ap, shape):
    shape = list(shape)
    if len(shape) == 1:
        return ap
    names = "abcd"[: len(shape)]
    kw = {n: v for n, v in zip(names[:-1], shape[:-1])}
    return ap.rearrange("p (%s) -> p %s" % (" ".join(names), " ".join(names)), **kw)


def host_consts():
    c = {}
    c["identf"] = np.eye(128, dtype=np.float32)
    s_ = np.arange(128)
    c["tri_cs"] = (s_[:, None] <= s_[None, :]).astype(np.float32)
    c["trimask"] = np.where(s_[:, None] <= s_[None, :], 0.0, NEGM).astype(np.float32)
    c["iota_row"] = np.tile(np.arange(128, dtype=np.float32)[None, :], (128, 1))
    c["iota_col"] = np.arange(128, dtype=np.float32)[:, None].copy()
    slopes = np.array([2.0 ** (-8.0 * (h + 1) / 8) for h in range(8)], np.float64)
    negc = np.zeros((128, 16, 8), np.float32)
    A = np.zeros((128, 16, 8), np.float32)
    Bc = np.zeros((128, 16, 8, 8), np.float32)
    for tt in range(16):
        own = tt // 2
        for n in range(8):
            negc[:, tt, n] = -1e30 if n >= own else 0.0
            A[:, tt, n] = 30000.0 if n < own else 0.0
        for h in range(8):
            cq = slopes[h] * (128 * (tt % 4) + np.arange(128))
            for n in range(8):
                Bc[:, tt, h, n] = (30000.0 if n == own else 0.0) - 30000.0 - cq
    c["negc"] = negc
    c["gA"] = A
    c["gB"] = np.ascontiguousarray(Bc.reshape(128, 16, 2, 4, 8).transpose(2, 0, 1, 3, 4))
    kb = np.zeros((128, 8, 16), np.float32)
    for h in range(8):
        for dd in range(16):
            kb[:, h, dd] = slopes[h] * (128 * (dd - 12) + np.arange(128))
    c["kbias"] = kb
    bi = np.zeros((8, 2048), np.float32)
    for n in range(8):
        bi[n, 256 * n:256 * (n + 1)] = 1.0
    c["blockind"] = bi.astype(ml_dtypes.bfloat16)
    cc = np.zeros((128, 8), np.float32)
    cc[:, 1] = 1.0
    cc[:, 2] = LN_EPS
    cc[:, 3] = RMS_EPS
    cc[:, 4] = math.pi / 2
    c["ccols"] = cc
    return c


CONST_SHAPES = {
    "identf": ([128, 128], F32), "tri_cs": ([128, 128], F32), "trimask": ([128, 128], F32),
    "iota_row": ([128, 128], F32), "iota_col": ([128, 1], F32), "negc": ([128, 16, 8], F32),
    "gA": ([128, 16, 8], F32), "gB": ([2, 128, 16, 4, 8], F32), "kbias": ([128, 8, 16], F32),
    "blockind": ([8, 2048], BF16), "ccols": ([128, 8], F32),
}

PARAM_SHAPES = {
    "ln0_g": [1024], "ln0_b": [1024], "w_in": [2, 1024, 2304], "mix_g": [2, 1024], "w_out": [2, 1024, 1024],
    "ssm_a_re": [2, 16, 64], "ssm_a_im": [2, 16, 64], "ssm_b_re": [2, 16, 64, 16], "ssm_b_im": [2, 16, 64, 16],
    "ssm_c_re": [2, 16, 16, 64], "ssm_c_im": [2, 16, 16, 64], "ssm_d": [2, 256], "ssm_log_dt": [2, 16],
    "ssm_w_glu": [2, 256, 256], "ssm_b_glu": [2, 256], "lru_conv_w": [2, 4, 256], "lru_conv_b": [2, 256],
    "lru_w_a": [2, 4, 64, 64], "lru_b_a": [2, 256], "lru_w_x": [2, 4, 64, 64], "lru_b_x": [2, 256],
    "lru_lam": [2, 256], "ln1_g": [2, 1024], "ln1_b": [2, 1024], "mem_wq": [2, 1024, 1024],
    "mem_wk": [2, 1024, 1024], "mem_wv": [2, 1024, 1024], "mem_wo": [2, 1024, 1024], "ln2_g": [2, 1024],
    "ln2_b": [2, 1024], "moe_wr_g": [2, 1024, 4], "moe_br_g": [2, 4], "moe_wr_e": [2, 1024, 16],
    "moe_br_e": [2, 16], "moe_w_gate": [2, 16, 1024, 256], "moe_w_up": [2, 16, 1024, 256],
    "moe_w_down": [2, 16, 256, 1024], "ln3_g": [2, 1024], "ln3_b": [2, 1024],
}


def build_program():
    nc = bass.Bass("TRN2", target_bir_lowering=False)
    Din = {}
    Din["x"] = nc.dram_tensor("x", [NTOK, D], F32, kind="ExternalInput").ap()
    Din["mem"] = nc.dram_tensor("mem", [512, D], F32, kind="ExternalInput").ap()
    for k, shp in PARAM_SHAPES.items():
        Din[k] = nc.dram_tensor(k, shp, F32, kind="ExternalInput").ap()
    for k, (shp, dt) in CONST_SHAPES.items():
        Din[k] = nc.dram_tensor(k, shp, dt, kind="ExternalInput").ap()
    out = nc.dram_tensor("out", [NTOK, D], F32, kind="ExternalOutput").ap()
    hbuf = nc.dram_tensor("hbuf", [NTOK, D], F32, kind="Internal").ap()
    PREPW = 10240
    prepbuf = nc.dram_tensor("prepbuf", [2, 128, PREPW], F32, kind="Internal").ap()
    dbg = {}
    if DEBUG:
        dbg["ya"] = nc.dram_tensor("dbg_ya", [NTOK, 512], F32, kind="ExternalOutput").ap()
        dbg["ys"] = nc.dram_tensor("dbg_ys", [NTOK, 256], F32, kind="ExternalOutput").ap()
        dbg["yl"] = nc.dram_tensor("dbg_yl", [256, NTOK], F32, kind="ExternalOutput").ap()
        dbg["h"] = nc.dram_tensor("dbg_h", [NTOK, D], F32, kind="ExternalOutput").ap()

    P = Prog(nc)
    st = ExitStack()
    SBW = 51200
    SBT = st.enter_context(nc.sbuf_tensor("SB", [128, SBW], F32))
    PSB = [st.enter_context(nc.psum_tensor("ps%d" % i, [128, 512], F32)) for i in range(8)]

    class Arena:
        def __init__(self):
            self.off = 0
            self.marks = []

        def f32(self, *shape):
            n = int(np.prod(shape))
            assert self.off + n <= SBW, ("SBUF overflow", self.off, n)
            ap = SBT[:, self.off:self.off + n]
            self.off += n
            return resh(ap, shape)

        def bf16(self, *shape):
            n = int(np.prod(shape))
            n32 = (n + 1) // 2
            assert self.off + n32 <= SBW, ("SBUF overflow", self.off, n32)
            ap = SBT[:, self.off:self.off + n32].bitcast(BF16)[:, 0:n]
            self.off += n32
            return resh(ap, shape)

        def i32(self, *shape):
            n = int(np.prod(shape))
            ap = SBT[:, self.off:self.off + n].bitcast(I32)
            self.off += n
            return resh(ap, shape)

        def mark(self):
            self.marks.append(self.off)

        def release(self, tag=None):
            if tag is not None and ARENA_LOG is not None:
                ARENA_LOG.append((tag, self.off * 4 // 1024))
            self.off = self.marks.pop()

    A = Arena()

    def psf(i, n=512):
        return PSB[i][:, 0:n]

    def psb(i, n=512):
        return PSB[i][:, 0:(n + 1) // 2].bitcast(BF16)[:, 0:n]

    def PK(i):
        return ("ps", i)

    def mm(o, lhsT, rhs, start, stop, r, w):
        P.op("pe", lambda e: e.matmul(o, lhsT=lhsT, rhs=rhs, start=start, stop=stop, skip_group_check=True), r, w)

    def tr(o, in_, ident, r, w):
        P.op("pe", lambda e: e.transpose(o, in_, ident), r, w)

    def act(o, in_, func, r, w, bias=None, scale=None, accum=None):
        kw = {}
        if bias is not None:
            kw["bias"] = bias
        if scale is not None:
            kw["scale"] = scale
        if accum is not None:
            kw["accum_out"] = accum
        P.op("act", lambda e: e.activation(out=o, in_=in_, func=func, **kw), r, w)

    def tt(o, a, b, op, r, w, eng="dve"):
        P.op(eng, lambda e: e.tensor_tensor(out=o, in0=a, in1=b, op=op), r, w)

    def ts(o, a, s1, s2, op0, op1, r, w, eng="dve", accum=None):
        if op1 is None:
            P.op(eng, lambda e: e.tensor_scalar(out=o, in0=a, scalar1=s1, scalar2=None, op0=op0), r, w)
        elif accum is not None:
            P.op(eng, lambda e: e.tensor_scalar(out=o, in0=a, scalar1=s1, scalar2=s2, op0=op0, op1=op1, accum_out=accum), r, w)
        else:
            P.op(eng, lambda e: e.tensor_scalar(out=o, in0=a, scalar1=s1, scalar2=s2, op0=op0, op1=op1), r, w)

    def stt(o, a, sc, b, op0, op1, r, w):
        P.op("dve", lambda e: e.scalar_tensor_tensor(out=o, in0=a, scalar=sc, in1=b, op0=op0, op1=op1), r, w)

    def cp(eng, o, in_, r, w):
        if eng == "act":
            P.op("act", lambda e: e.copy(out=o, in_=in_), r, w)
        else:
            P.op(eng, lambda e: e.tensor_copy(out=o, in_=in_), r, w)

    def memset(eng, o, val, w):
        P.op(eng, lambda e: e.memset(o, val), (), w)

    def dma(q, o, in_, r, w, slow=False, maxlast=None):
        kw = {}
        if slow:
            kw["allow_slow_non_contiguous"] = True
        if maxlast is not None:
            kw["max_dma_last_dim"] = maxlast
        P.dma(q, lambda e: e.dma_start(out=o, in_=in_, **kw), r, w)

    def recip(o, in_, r, w):
        P.op("dve", lambda e: e.reciprocal(out=o, in_=in_), r, w)

    hT = A.bf16(8, S)
    CUR = {"T0": 0}
    identf = A.f32(128)
    identb = A.bf16(128)
    tri_cs = A.bf16(128)
    trimask = A.bf16(128)
    onesb = A.bf16(128)
    iota_row = A.f32(128)
    iota_col = A.f32(1)
    ccols = A.f32(8)
    tmpc = A.f32(128)
    C0, C1, CEPS, CRMS, CHPI = (ccols[:, i:i + 1] for i in range(5))

    dma("sp", identf, Din["identf"], (), ["identf"])
    dma("sp", tmpc, Din["tri_cs"], (), ["tmpc"])
    cp("dve", tri_cs, tmpc, ["tmpc"], ["tri_cs"])
    cp("dve", identb, identf, ["identf"], ["identb"])
    dma("sp", tmpc, Din["trimask"], ["tmpc"], ["tmpc"])
    cp("dve", trimask, tmpc, ["tmpc"], ["trimask"])
    dma("sp", iota_row, Din["iota_row"], (), ["iota_row"])
    dma("sp", iota_col, Din["iota_col"], (), ["iota_col"])
    dma("sp", ccols, Din["ccols"], (), ["ccols"])
    memset("dve", onesb, 1.0, ["onesb"])
    P.barrier()

    def ln_tile(acc, kacc, gt, bt, tok0, slot, final=False, psbanks=(6, 7), dbgh=False, defer=False, geng="pool", staged=False, tcopy=None):
        st6, mv, sd, rstd, nmr, yt, y2 = ln_tmp[slot]
        k = lambda n: ("ln", n, slot)
        tl_ = tok0 - CUR["T0"]
        kyt = k("yt")

        def s_a():
            P.op("dve", lambda e: e.bn_stats(out=st6[:, 0, :], in_=acc[:, 0:512]), [kacc], [k("st0")])
            P.op("dve", lambda e: e.bn_stats(out=st6[:, 1, :], in_=acc[:, 512:1024]), [kacc], [k("st1")])
            P.op("dve", lambda e: e.bn_aggr(out=mv, in_=st6.rearrange("p a b -> p (a b)")), [k("st0"), k("st1")], [k("mv")])
            act(sd, mv[:, 1:2], AF.Sqrt, [k("mv")], [k("sd")], bias=CEPS, scale=1.0)

        def s_b():
            recip(rstd, sd, [k("sd")], [k("rstd")])
            stt(nmr, mv[:, 0:1], -1.0, rstd, ALU.mult, ALU.mult, [k("mv"), k("rstd")], [k("nmr")])
            act(yt, acc, AF.Identity, [kacc, k("rstd"), k("nmr")], [k("yt")], bias=nmr, scale=rstd)

        def s_c():
            tt(y2, yt, gt, ALU.mult, [k("yt"), "lng"], [k("y2")], eng=geng)
            tt(yt, y2, bt, ALU.add, [k("y2"), "lnb"], [k("yt")], eng=geng)
            if final:
                dma("sp", out[tok0:tok0 + 128, :], yt, [k("yt")], [("out", tok0)])
                return
            dma("sp", hbuf[tok0:tok0 + 128, :], yt, [k("yt")], [("hbuf", tok0)])
            if dbgh and DEBUG:
                dma("sp", dbg["h"][tok0:tok0 + 128, :], yt, [k("yt")], [("dbgh", tok0)])

        def emit_tr(pb=None):
            if final:
                return
            banks = psbanks if pb is None else pb
            for half in range(2):
                b = banks[half]
                for c in range(4):
                    tr(PSB[b][:, 128 * c:128 * c + 128], yt[:, 512 * half + 128 * c: 512 * half + 128 * c + 128], identf,
                       [kyt, "identf"], [PK(b)])
                o = hT[:, 4 * half:4 * half + 4, tl_:tl_ + 128]
                cp("act" if half == 0 else "dve", o, resh(psf(b), [4, 128]), [PK(b)], [("hT", tok0 // 128), ("psrd", b)])
                if tcopy is not None:
                    cp("dve" if half == 0 else "act", tcopy[0][:, 4 * half:4 * half + 4, :], resh(psf(b), [4, 128]),
                       [PK(b), ("psrd", b), tcopy[1]], [tcopy[1]])

        if staged:
            return [s_a, s_b, s_c, emit_tr]
        s_a(); s_b(); s_c()
        if final:
            return (lambda: None)
        if defer:
            return emit_tr
        emit_tr()
        return (lambda: None)

    def ln_pipeline(n, make, pre=None, hook_b=None):
        stg = [None] * n
        for it in range(n + 3):
            if it < n:
                if pre is not None:
                    pre(it)
                stg[it] = make(it)
                stg[it][0]()
            if 0 <= it - 1 < n:
                stg[it - 1][1]()
                if hook_b is not None:
                    hook_b(it - 1)
            if 0 <= it - 2 < n:
                stg[it - 2][2]()
            if 0 <= it - 3 < n:
                stg[it - 3][3]()

    def load_ln(gname, bname, l):
        g_ap = Din[gname] if l is None else Din[gname][l]
        b_ap = Din[bname] if l is None else Din[bname][l]
        dma("sp", lng, g_ap.partition_broadcast(128), (), ["lng"])
        dma("sp", lnb, b_ap.partition_broadcast(128), (), ["lnb"])

    def cexp_table(o_re, o_im, lr, li, shape, sign, tmp, kp):
        mag, th, kf, sn, ki = tmp
        r = lambda *n: [(kp, x) for x in n]
        act(mag, lr, AF.Exp, r("lr"), r("mag"), scale=float(sign))
        for which, o in ((0, o_im), (1, o_re)):
            ts(th, li, float(sign), (math.pi / 2 if which else 0.0), ALU.mult, ALU.add, r("li"), r("th"))
            ts(kf, th, 1.0 / TWO_PI, None, ALU.mult, None, r("th"), r("kf"))
            cp("dve", ki, kf, r("kf"), r("ki"))
            cp("dve", kf, ki, r("ki"), r("kf"))
            stt(th, kf, -TWO_PI, th, ALU.mult, ALU.add, r("kf", "th"), r("th"))
            ts(th, th, 3.14159, -3.14159, ALU.min, ALU.max, r("th"), r("th"))
            act(sn, th, AF.Sin, r("th"), r("sn"))
            tt(o, sn, mag, ALU.mult, r("sn", "mag"), r("o%d" % which))

    for SQ in range(2):
        CUR["T0"] = S * SQ
        A.mark()
        lng = A.f32(1024)
        lnb = A.f32(1024)
        ln_tmp = [(A.f32(2, 6), A.f32(2), A.f32(1), A.f32(1), A.f32(1), A.f32(1024), A.f32(1024)) for _ in range(4)]
        xt = [A.f32(1024) for _ in range(4)]
        load_ln("ln0_g", "ln0_b", None)
        t_lo, t_hi = 16 * SQ, 16 * SQ + 16
        for t in range(t_lo, t_lo + 4):
            dma("sp", xt[t % 4], Din["x"][128 * t:128 * t + 128, :], (), [("xt", t % 4)])

        def ln0_hook(i):
            t = t_lo + i + 4
            if t < t_hi:
                dma("sp", xt[t % 4], Din["x"][128 * t:128 * t + 128, :], (), [("xt", t % 4)])

        ln_pipeline(16, lambda i: ln_tile(xt[(t_lo + i) % 4], ("xt", (t_lo + i) % 4), lng, lnb, 128 * (t_lo + i), (t_lo + i) % 4, staged=True, geng="dve"),
                    hook_b=ln0_hook)
        A.release("ln0")
        P.barrier()

        for l in range(2):
            A.mark()
            A.mark()
            L = "L%d" % l
            Wu = A.bf16(8, 256)
            dma("pool", Wu, Din["w_in"][l].rearrange("(kc p) n -> p kc n", p=128)[:, :, 1536:1792], (), ["Wu"])
            LP0 = A.off
            P_re = A.f32(1024); P_im = A.f32(1024)
            Q_re = A.f32(8, 128); Q_im = A.f32(8, 128)
            Q1_re = A.bf16(8, 128); Q1_im = A.bf16(8, 128)
            AL_re = A.f32(8); AL_im = A.f32(8)
            AB_re = A.f32(8); AB_im = A.f32(8)
            Bbig = A.bf16(2, 2, 512)
            Cst_re = A.f32(8, 32); Cst_nim = A.f32(8, 32)
            Cst_re_b = A.bf16(8, 32); Cst_nim_b = A.bf16(8, 32)
            Ddiag = A.bf16(2, 128)
            wglu = A.bf16(2, 256)
            bglu_b = A.f32(256)
            WAbd = A.bf16(2, 128); WXbd = A.bf16(2, 128)
            lrup = A.f32(2, 12)
            mixg_b = A.f32(1024)
            LP1 = A.off
            P_real = P
            if SQ == 1:
                P = Prog(nc)
            A.mark()
            lam_tm = A.f32(2, 1024)
            dt_tm = A.f32(1024)
            tmpA = (A.f32(1024), A.f32(1024), A.f32(1024), A.f32(1024), A.i32(1024))
            prm = A.f32(8, 8)
            Bst = A.f32(2, 8, 16)
            Bbar = A.f32(2, 8, 16)
            Bwide = A.f32(2, 8, 128)
            Cwide = A.f32(2, 8, 128)
            fcoef = A.f32(8, 8)
            wtmp = A.f32(2, 256)
            wbd = A.f32(2, 2, 128)
            dcol = A.f32(2)
            are_d = Din["ssm_a_re"][l]; aim_d = Din["ssm_a_im"][l]; ldt_d = Din["ssm_log_dt"][l]
            flat = lambda ap_: ap_.rearrange("g p -> (g p)")
            dma("sp", lam_tm[:, 0, :], flat(are_d).partition_broadcast(128), (), [(L, "lamtm")])
            dma("sp", lam_tm[:, 1, :], flat(aim_d).partition_broadcast(128), (), [(L, "lamtm")])
            ldt_t = ldt_d.tensor
            ldt16 = A.f32(16)
            dma("sp", ldt16, ldt_d.partition_broadcast(128), (), [(L, "ldt16")])
            act(ldt16, ldt16, AF.Exp, [(L, "ldt16")], [(L, "ldt16")])
            cp("dve", resh(dt_tm, [16, 64]), ldt16.unsqueeze(2).to_broadcast([128, 16, 64]), [(L, "ldt16")], [(L, "dttm")])
            tt(lam_tm[:, 0, :], lam_tm[:, 0, :], dt_tm, ALU.mult, [(L, "lamtm"), (L, "dttm")], [(L, "lamtm")])
            tt(lam_tm[:, 1, :], lam_tm[:, 1, :], dt_tm, ALU.mult, [(L, "lamtm"), (L, "dttm")], [(L, "lamtm")])
            ts(lam_tm[:, 0, :], lam_tm[:, 0, :], iota_col, None, ALU.mult, None, [(L, "lamtm")], [(L, "lamtm")])
            ts(lam_tm[:, 1, :], lam_tm[:, 1, :], iota_col, None, ALU.mult, None, [(L, "lamtm")], [(L, "lamtm")])
            P.op("dve", lambda e: e.tensor_copy(out=tmpA[0][:, 0:1], in_=lam_tm[:, 0, 0:1]), [(L, "lamtm")], [("cxP", "lr"), ("cxP", "li")])
            cexp_table(P_re, P_im, lam_tm[:, 0, :], lam_tm[:, 1, :], None, -1.0, tmpA, "cxP")
            fm = lambda ap_: ap_.rearrange("g p -> (g p)").rearrange("(k q) -> q k", q=128)
            dma("sp", prm[:, :, 0], fm(are_d), (), [(L, "prm")], slow=True)
            dma("sp", prm[:, :, 1], fm(aim_d), (), [(L, "prm")], slow=True)
            for half in range(2):
                srcd = bass.AP(ldt_t, ldt_d.offset + half, [[0, 64], [2, 8]])
                dma("sp", prm[64 * half:64 * half + 64, :, 2], srcd, (), [(L, "prm")], slow=True)
            act(prm[:, :, 3], prm[:, :, 2], AF.Exp, [(L, "prm")], [(L, "prm")])
            tt(prm[:, :, 4], prm[:, :, 0], prm[:, :, 3], ALU.mult, [(L, "prm")], [(L, "prm")])
            tt(prm[:, :, 5], prm[:, :, 1], prm[:, :, 3], ALU.mult, [(L, "prm")], [(L, "prm")])
            lre = prm[:, :, 4]; lim = prm[:, :, 5]
            argr = resh(tmpA[0], [8, 128]); argi = resh(tmpA[1], [8, 128])
            qa_r = A.f32(8, 128); qa_i = A.f32(8, 128)
            tmpB = (A.f32(1024), A.f32(1024), A.f32(1024), A.f32(1024), A.i32(1024))
            irb = iota_row.unsqueeze(1).to_broadcast([128, 8, 128])
            tt(qa_r, irb, lre.unsqueeze(2).to_broadcast([128, 8, 128]), ALU.mult, [(L, "prm"), "iota_row"], [("cxQ", "lr")])
            tt(qa_i, irb, lim.unsqueeze(2).to_broadcast([128, 8, 128]), ALU.mult, [(L, "prm"), "iota_row"], [("cxQ", "li")])
            cexp_table(Q_re.rearrange("p a b -> p (a b)"), Q_im.rearrange("p a b -> p (a b)"),
                       qa_r.rearrange("p a b -> p (a b)"), qa_i.rearrange("p a b -> p (a b)"), None, 1.0, tmpB, "cxQ")
            sm = [A.f32(8) for _ in range(4)] + [A.i32(8)]
            cp("dve", sm[0][:, 0:1], lre[:, 0:1], [(L, "prm")], [("cxA", "lr"), ("cxA", "li")])
            cexp_table(AB_re, AB_im, lre, lim, None, 1.0, sm, "cxA")
            l128 = A.f32(2, 8)
            ts(l128[:, 0, :], lre, 128.0, None, ALU.mult, None, [(L, "prm")], [("cxL", "lr")])
            ts(l128[:, 1, :], lim, 128.0, None, ALU.mult, None, [(L, "prm")], [("cxL", "li")])
            sm2 = [A.f32(8) for _ in range(4)] + [A.i32(8)]
            cexp_table(AL_re, AL_im, l128[:, 0, :], l128[:, 1, :], None, 1.0, sm2, "cxL")
            t1 = resh(tmpB[0], [8, 128]); t2 = resh(tmpB[1], [8, 128])
            abr = AB_re.unsqueeze(2).to_broadcast([128, 8, 128]); abi = AB_im.unsqueeze(2).to_broadcast([128, 8, 128])
            qk = [("cxQ", "o0"), ("cxQ", "o1"), ("cxA", "o0"), ("cxA", "o1"), ("cxQ", "sn"), ("cxQ", "mag")]
            tt(t1, Q_re, abr, ALU.mult, qk, [(L, "t1")])
            tt(t2, Q_im, abi, ALU.mult, qk, [(L, "t2")])
            tt(Q1_re, t1, t2, ALU.subtract, [(L, "t1"), (L, "t2")], [(L, "Q1")])
            tt(t1, Q_re, abi, ALU.mult, qk + [(L, "Q1")], [(L, "t1")])
            tt(t2, Q_im, abr, ALU.mult, qk + [(L, "Q1")], [(L, "t2")])
            tt(Q1_im, t1, t2, ALU.add, [(L, "t1"), (L, "t2")], [(L, "Q1")])
            are = prm[:, :, 0]; aim = prm[:, :, 1]
            fk = [(L, "prm"), ("cxA", "o0"), ("cxA", "o1"), (L, "fc")]
            f = lambda i: fcoef[:, :, i]
            ts(f(2), AB_re, -1.0, None, ALU.add, None, fk, [(L, "fc")])
            tt(f(3), are, are, ALU.mult, fk, [(L, "fc")])
            tt(f(4), aim, aim, ALU.mult, fk, [(L, "fc")])
            tt(f(3), f(3), f(4), ALU.add, fk, [(L, "fc")])
            recip(f(3), f(3), fk, [(L, "fc")])
            tt(f(4), f(2), are, ALU.mult, fk, [(L, "fc")])
            tt(f(5), AB_im, aim, ALU.mult, fk, [(L, "fc")])
            tt(f(4), f(4), f(5), ALU.add, fk, [(L, "fc")])
            tt(f(0), f(4), f(3), ALU.mult, fk, [(L, "fc")])
            tt(f(4), AB_im, are, ALU.mult, fk, [(L, "fc")])
            tt(f(5), f(2), aim, ALU.mult, fk, [(L, "fc")])
            tt(f(4), f(4), f(5), ALU.subtract, fk, [(L, "fc")])
            tt(f(1), f(4), f(3), ALU.mult, fk, [(L, "fc")])
            bsrc = lambda ap_: ap_.rearrange("(k gl) p h -> (gl p) k h", gl=2)
            dma("sp", Bst[:, 0], bsrc(Din["ssm_b_re"][l]), (), [(L, "Bst")])
            dma("sp", Bst[:, 1], bsrc(Din["ssm_b_im"][l]), (), [(L, "Bst")])
            frb = f(0).unsqueeze(2).to_broadcast([128, 8, 16]); fib = f(1).unsqueeze(2).to_broadcast([128, 8, 16])
            u1 = resh(tmpB[2][:, 0:128], [8, 16]); u2 = resh(tmpB[3][:, 0:128], [8, 16])
            bk = [(L, "Bst"), (L, "fc"), (L, "u1"), (L, "u2"), (L, "Bbar")]
            tt(u1, Bst[:, 0], frb, ALU.mult, bk, [(L, "u1")])
            tt(u2, Bst[:, 1], fib, ALU.mult, bk, [(L, "u2")])
            tt(Bbar[:, 0], u1, u2, ALU.subtract, bk, [(L, "Bbar")])
            tt(u1, Bst[:, 1], frb, ALU.mult, bk, [(L, "u1")])
            tt(u2, Bst[:, 0], fib, ALU.mult, bk, [(L, "u2")])
            tt(Bbar[:, 1], u1, u2, ALU.add, bk, [(L, "Bbar")])
            memset("pool", Bwide.rearrange("p a b c -> p (a b c)"), 0.0, [(L, "Bwide")])
            bw_t = Bwide.tensor
            for ri in range(2):
                for gl in range(2):
                    base = Bwide.offset + (64 * gl) * SBW + ri * 1024 + 16 * gl
                    o = bass.AP(bw_t, base, [[SBW, 64], [512, 2], [160, 4], [1, 16]])
                    i_ = resh(Bbar[64 * gl:64 * gl + 64, ri].rearrange("p k h -> p (k h)"), [2, 4, 16])
                    cp("dve", o, i_, [(L, "Bbar"), (L, "Bwide")], [(L, "Bwide")])
            for ri in range(2):
                for k in range(8):
                    j, kl = k // 4, k % 4
                    b = 4 + (k % 2)
                    tr(PSB[b][:, 0:128], Bwide[:, ri, k, :], identf, [(L, "Bwide"), "identf"], [PK(b)])
                    cp("act", Bbig[:, ri, j, 128 * kl:128 * kl + 128], PSB[b][:, 0:128], [PK(b)], [(L, "Bbig")])
            memset("pool", Cwide.rearrange("p a b c -> p (a b c)"), 0.0, [(L, "Cwide")])
            for ri, nm in ((0, "ssm_c_re"), (1, "ssm_c_im")):
                cd = Din[nm][l]
                for gl in range(2):
                    src_ = cd.rearrange("(k gl) h p -> gl h k p", gl=2)[gl]
                    dma("sp", Cwide[16 * gl:16 * gl + 16, ri, :, 64 * gl:64 * gl + 64], src_, [(L, "Cwide")], [(L, "Cwide")])
            for ri in range(2):
                for k in range(8):
                    b = 4 + (k % 2)
                    tr(PSB[b][:, 0:32], Cwide[0:32, ri, k, :], identf[0:32, 0:32], [(L, "Cwide"), "identf"], [PK(b)])
                    if ri == 0:
                        cp("act", Cst_re[:, k, :], PSB[b][:, 0:32], [PK(b)], [(L, "Cst")])
                    else:
                        act(Cst_nim[:, k, :], PSB[b][:, 0:32], AF.Copy, [PK(b)], [(L, "Cst")], scale=-1.0)
            cp("dve", Cst_re_b, Cst_re, [(L, "Cst")], [(L, "Cstb")])
            cp("dve", Cst_nim_b, Cst_nim, [(L, "Cst")], [(L, "Cstb")])
            dma("sp", dcol, Din["ssm_d"][l].rearrange("(j p) -> p j", p=128), (), [(L, "dcol")], slow=True)
            for j in range(2):
                ts(Ddiag[:, j, :], identf, dcol[:, j:j + 1], None, ALU.mult, None, [(L, "dcol"), "identf"], [(L, "Ddiag")])
            dma("pool", wglu, Din["ssm_w_glu"][l].rearrange("(j p) n -> p j n", p=128), (), [(L, "wglu")])
            dma("sp", bglu_b, Din["ssm_b_glu"][l].partition_broadcast(128), (), [(L, "bglu")])
            pcol = lambda nm: Din[nm][l].rearrange("(j p) -> p j", p=128)
            for jj in range(4):
                dma("sp", lrup[:, :, jj], Din["lru_conv_w"][l][jj].rearrange("(j p) -> p j", p=128), (), [(L, "lrup")], slow=True)
            for ci, nm in ((4, "lru_conv_b"), (5, "lru_b_a"), (6, "lru_b_x"), (7, "lru_lam")):
                dma("sp", lrup[:, :, ci], pcol(nm), (), [(L, "lrup")], slow=True)
            dma("sp", lrup[:, :, 9], Din["mix_g"][l][768:1024].rearrange("(j p) -> p j", p=128), (), [(L, "lrup")], slow=True)
            act(lrup[:, :, 10], lrup[:, :, 7], AF.Exp, [(L, "lrup")], [(L, "lrup")], scale=-1.0)
            act(lrup[:, :, 10], lrup[:, :, 10], AF.Ln, [(L, "lrup")], [(L, "lrup")], bias=C1, scale=1.0)
            ts(lrup[:, :, 8], lrup[:, :, 10], -8.0, None, ALU.mult, None, [(L, "lrup")], [(L, "lrup")])
            ts(lrup[:, :, 11], lrup[:, :, 8], 2.0, None, ALU.mult, None, [(L, "lrup")], [(L, "lrup")])
            memset("pool", wbd.rearrange("p a b c -> p (a b c)"), 0.0, [(L, "wbd")])
            for wi, nm in ((0, "lru_w_a"), (1, "lru_w_x")):
                for hh in range(4):
                    fc, hl = hh // 2, hh % 2
                    dma("sp", wbd[64 * hl:64 * hl + 64, wi, fc, 64 * hl:64 * hl + 64], Din[nm][l][hh], [(L, "wbd")], [(L, "wbd")])
            cp("dve", WAbd, wbd[:, 0], [(L, "wbd")], [(L, "WAbd")])
            cp("dve", WXbd, wbd[:, 1], [(L, "wbd")], [(L, "WAbd")])
            dma("sp", mixg_b, Din["mix_g"][l].partition_broadcast(128), (), [(L, "mixgb")])
            P.barrier()
            A.release("prep")
            P = P_real
            assert LP1 - LP0 <= PREPW, (LP1 - LP0)
            if SQ == 0:
                dma("sp", prepbuf[l][:, 0:LP1 - LP0], SBT[:, LP0:LP1], (), [("prepbuf", l)])
            else:
                dma("sp", SBT[:, LP0:LP1], prepbuf[l][:, 0:LP1 - LP0], [("prepbuf", l)], [("prepld", l)])
                P.barrier()

            for s in (SQ,):
                T0 = S * s
                A.mark()
                Yattn = A.bf16(16, 512)
                Yssm = A.bf16(16, 256)
                Ylru = A.bf16(16, 256)
                ssqS = A.f32(16)
                A.mark()
                Wqkv0 = [A.bf16(8, 256) for _ in range(3)]
                Wxl2 = [A.bf16(8, 128) for _ in range(2)]; Wgl2 = [A.bf16(8, 128) for _ in range(2)]
                A.mark()
                uT = A.bf16(2, S)
                paB = [[A.bf16(1024) for _ in range(4)] for _ in range(2)]
                vB = [[A.bf16(8, 128) for _ in range(4)] for _ in range(2)]
                car = A.f32(2, 8)
                cart = [A.f32(8) for _ in range(4)]
                E12 = A.f32(2, 8, 32)
                E12b = A.bf16(2, 8, 32)
                ygl = [A.f32(256) for _ in range(2)]
                yglT = [A.bf16(2, 128) for _ in range(2)]
                sg = [A.f32(256) for _ in range(2)]
                w3 = Din["w_in"][l].rearrange("(kc p) n -> p kc n", p=128)
                for fc in range(2):
                    dma("pool", Wxl2[fc], w3[:, :, 1792 + 128 * fc:1792 + 128 * fc + 128], (), [("Wxl", fc)])
                    dma("pool", Wgl2[fc], w3[:, :, 2048 + 128 * fc:2048 + 128 * fc + 128], (), [("Wgl", fc)])
                for wi_ in range(3):
                    dma("pool", Wqkv0[wi_], w3[:, :, 512 * wi_:512 * wi_ + 256], (), [("Wqkv", 0)])
                for fc in range(2):
                    for tg in range(4):
                        b = (fc * 4 + tg) % 2
                        for kc in range(8):
                            mm(PSB[b][:, :], Wu[:, kc, 128 * fc:128 * fc + 128], hT[:, kc, 512 * tg:512 * tg + 512], kc == 0, kc == 7,
                               ["Wu", ("hT", 0)], [PK(b)])
                        cp("act" if tg % 2 == 0 else "dve", uT[:, fc, 512 * tg:512 * tg + 512], PSB[b][:, :], [PK(b)], ["uT"])
                memset("dve", car.rearrange("p a b -> p (a b)"), 0.0, ["car"])

                def s5_A(c):
                    sl = c % 2
                    tk = slice(128 * c, 128 * c + 128)
                    for ri in range(2):
                        for j in range(2):
                            bnk = 2 * ri + j
                            mm(PSB[bnk][:, :], uT[:, j, tk], Bbig[:, ri, j, :], True, True, ["uT", (L, "Bbig")], [PK(bnk)])
                    for j in range(2):
                        cs = slice(512 * j, 512 * j + 512)
                        bur, bui = PSB[j][:, :], PSB[2 + j][:, :]
                        tt(paB[sl][0][:, cs], bur, P_re[:, cs], ALU.mult, [PK(j)], [("pa", sl, 0, j)])
                        stt(paB[sl][1][:, cs], bui, -1.0, P_im[:, cs], ALU.mult, ALU.mult, [PK(2 + j)], [("pa", sl, 1, j)])
                        tt(paB[sl][2][:, cs], bur, P_im[:, cs], ALU.mult, [PK(j)], [("pa", sl, 2, j)])
                        tt(paB[sl][3][:, cs], bui, P_re[:, cs], ALU.mult, [PK(2 + j)], [("pa", sl, 3, j)])

                def s5_B(c):
                    sl = c % 2
                    for ri in range(2):
                        ia, ib = (0, 1) if ri == 0 else (2, 3)
                        for k in range(8):
                            bnk = 4 + 2 * ri + k // 4
                            o_ = PSB[bnk][:, 128 * (k % 4):128 * (k % 4) + 128]
                            mm(o_, paB[sl][ia][:, 128 * k:128 * k + 128], tri_cs, True, False, [("pa", sl, ia, k // 4), "tri_cs"], [PK(bnk)])
                            mm(o_, paB[sl][ib][:, 128 * k:128 * k + 128], tri_cs, False, True, [("pa", sl, ib, k // 4), "tri_cs"], [PK(bnk)])
                    for hf in range(2):
                        zr = resh(PSB[4 + hf][:, :], [4, 128]); zi = resh(PSB[6 + hf][:, :], [4, 128])
                        ks = slice(4 * hf, 4 * hf + 4)
                        tt(vB[sl][0][:, ks, :], zr, Q_re[:, ks, :], ALU.mult, [PK(4 + hf)], [("vB", sl, 0, hf)])
                        stt(vB[sl][1][:, ks, :], zi, -1.0, Q_im[:, ks, :], ALU.mult, ALU.mult, [PK(6 + hf)], [("vB", sl, 1, hf)])
                        tt(vB[sl][2][:, ks, :], zr, Q_im[:, ks, :], ALU.mult, [PK(4 + hf)], [("vB", sl, 2, hf)])
                        tt(vB[sl][3][:, ks, :], zi, Q_re[:, ks, :], ALU.mult, [PK(6 + hf)], [("vB", sl, 3, hf)])

                def s5_Cpre(c):
                    xr = car[:, 0, :].unsqueeze(2).to_broadcast([128, 8, 32]); xi = car[:, 1, :].unsqueeze(2).to_broadcast([128, 8, 32])
                    ek = ["car", "E12", (L, "Cst")]
                    tt(E12[:, 0], Cst_re, xr, ALU.mult, ek, ["E12"], eng="pool")
                    tt(E12[:, 1], Cst_nim, xi, ALU.mult, ek, ["E12"], eng="pool")
                    tt(E12b[:, 0], E12[:, 0], E12[:, 1], ALU.add, ["E12"], ["E12b"], eng="pool")
                    tt(E12[:, 0], Cst_re, xi, ALU.mult, ek, ["E12"], eng="pool")
                    tt(E12[:, 1], Cst_nim, xr, ALU.mult, ek, ["E12"], eng="pool")
                    tt(E12b[:, 1], E12[:, 1], E12[:, 0], ALU.subtract, ["E12"], ["E12b"], eng="pool")

                def s5_C(c):
                    sl = c % 2
                    tk = slice(128 * c, 128 * c + 128)
                    XTk = [("vB", sl, i_, hf_) for i_ in range(4) for hf_ in range(2)]
                    yb = 0
                    for j in range(2):
                        mm(PSB[yb][:, 128 * j:128 * j + 128], uT[:, j, tk], Ddiag[:, j, :], True, False, ["uT", (L, "Ddiag")], [PK(yb)])
                        for kk in range(4):
                            k = 4 * j + kk
                            oc = PSB[yb][:, 32 * k:32 * k + 32]
                            mm(oc, vB[sl][0][:, k, :], Cst_re_b[:, k, :], False, False, XTk + [(L, "Cstb")], [PK(yb)])
                            mm(oc, vB[sl][1][:, k, :], Cst_re_b[:, k, :], False, False, XTk + [(L, "Cstb")], [PK(yb)])
                            mm(oc, vB[sl][2][:, k, :], Cst_nim_b[:, k, :], False, False, XTk + [(L, "Cstb")], [PK(yb)])
                            mm(oc, vB[sl][3][:, k, :], Cst_nim_b[:, k, :], False, False, XTk + [(L, "Cstb")], [PK(yb)])
                            mm(oc, Q1_re[:, k, :], E12b[:, 0, k, :], False, False, [(L, "Q1"), "E12b"], [PK(yb)])
                            mm(oc, Q1_im[:, k, :], E12b[:, 1, k, :], False, kk == 3, [(L, "Q1"), "E12b"], [PK(yb)])
                    vl = [vB[sl][i_][:, :, 127] for i_ in range(4)]
                    ck = ["car", "cart", ("cxL", "o0"), ("cxL", "o1")] + XTk
                    tt(cart[0], AL_re, car[:, 0, :], ALU.mult, ck, ["cart"], eng="pool")
                    tt(cart[1], AL_im, car[:, 1, :], ALU.mult, ck, ["cart"], eng="pool")
                    tt(cart[2], AL_re, car[:, 1, :], ALU.mult, ck, ["cart"], eng="pool")
                    tt(cart[3], AL_im, car[:, 0, :], ALU.mult, ck, ["cart"], eng="pool")
                    tt(cart[0], cart[0], cart[1], ALU.subtract, ["cart"], ["cart"], eng="pool")
                    tt(cart[2], cart[2], cart[3], ALU.add, ["cart"], ["cart"], eng="pool")
                    tt(cart[0], cart[0], vl[0], ALU.add, ["cart"] + XTk, ["cart"], eng="pool")
                    tt(cart[2], cart[2], vl[2], ALU.add, ["cart"] + XTk, ["cart"], eng="pool")
                    tt(car[:, 0, :], cart[0], vl[1], ALU.add, ["cart", "E12"] + XTk, ["car"], eng="pool")
                    tt(car[:, 1, :], cart[2], vl[3], ALU.add, ["cart", "E12"] + XTk, ["car"], eng="pool")
                    act(ygl[sl], PSB[yb][:, 0:256], AF.Gelu_apprx_tanh, [PK(yb)], [("ygl", sl)])
                    tb = 1
                    for j in range(2):
                        tr(PSB[tb][:, 128 * j:128 * j + 128], ygl[sl][:, 128 * j:128 * j + 128], identf, [("ygl", sl), "identf"], [PK(tb)])
                    cp("act", yglT[sl], resh(PSB[tb][:, 0:256], [2, 128]), [PK(tb)], [("yglT", sl)])
                    gb2 = 2
                    for j in range(2):
                        mm(PSB[gb2][:, 0:256], yglT[sl][:, j, :], wglu[:, j, :], j == 0, j == 1, [("yglT", sl), (L, "wglu")], [PK(gb2)])
                    tt(sg[sl], PSB[gb2][:, 0:256], bglu_b, ALU.add, [PK(gb2), (L, "bglu")], [("sg", sl)])
                    act(sg[sl], sg[sl], AF.Sigmoid, [("sg", sl)], [("sg", sl)])

                def s5_Cpost(c):
                    sl = c % 2
                    tt(ygl[sl], ygl[sl], sg[sl], ALU.mult, [("ygl", sl), ("sg", sl), ("yglT", sl)], [("ygl", sl)], eng="pool")
                    cp("pool", Yssm[:, c, :], ygl[sl], [("ygl", sl)], [("Yssm", c)])
                    act(sg[sl], ygl[sl], AF.Square, [("ygl", sl), ("sg", sl)], [("sg", sl)], accum=ssqS[:, c:c + 1])
                    if DEBUG and l == 0:
                        dma("sp", dbg["ys"][T0 + 128 * c:T0 + 128 * c + 128, :], ygl[sl], [("ygl", sl)], [("dbgys", c)])

                s5_A(0)
                for c in range(18):
                    if 0 <= c - 1 < 16:
                        s5_Cpre(c - 1)
                    if 0 <= c - 2 < 16:
                        s5_Cpost(c - 2)
                    if c + 1 < 16:
                        s5_A(c + 1)
                    if c < 16:
                        s5_B(c)
                    if 0 <= c - 1 < 16:
                        s5_C(c - 1)
                A.release("s5")
                P.barrier()

                A.mark()
                xpad2 = [A.f32(S + 4) for _ in range(2)]
                glg2 = [A.bf16(S) for _ in range(2)]
                xcb2 = [A.bf16(S) for _ in range(2)]
                cacc = A.f32(512)
                rr = [A.f32(512) for _ in range(2)]
                ii = [A.f32(512) for _ in range(2)]
                aa = [[A.f32(512) for _ in range(2)] for _ in range(2)]
                bb = [[A.f32(512) for _ in range(2)] for _ in range(2)]
                a2 = [A.f32(512) for _ in range(2)]
                hs2 = [[A.f32(512) for _ in range(2)] for _ in range(2)]
                hg_ = [A.f32(512) for _ in range(2)]
                w3 = Din["w_in"][l].rearrange("(kc p) n -> p kc n", p=128)
                for fc in range(2):
                    memset("dve", xpad2[fc][:, 0:4], 0.0, [("xpad", fc)])
                for fc in range(2):
                    xpad = xpad2[fc]; glg = glg2[fc]; xcb = xcb2[fc]
                    pc = lambda i, fc=fc: lrup[:, fc, i:i + 1]
                    for tg in range(4):
                        for which, W_ in ((0, Wxl2[fc]), (1, Wgl2[fc])):
                            b = which
                            for kc in range(8):
                                mm(PSB[b][:, :], W_[:, kc, :], hT[:, kc, 512 * tg:512 * tg + 512], kc == 0, kc == 7,
                                   [("Wxl", fc), ("Wgl", fc)], [PK(b)])
                            if which == 0:
                                cp("dve", xpad[:, 4 + 512 * tg:4 + 512 * tg + 512], PSB[b][:, :], [PK(b), ("xpad", fc)], [("xpad", fc)])
                            else:
                                act(glg[:, 512 * tg:512 * tg + 512], PSB[b][:, :], AF.Gelu_apprx_tanh, [PK(b)], [("glg", fc)])
                    for tg in range(4):
                        tk = slice(512 * tg, 512 * tg + 512)
                        ts(cacc, xpad[:, 4 + 512 * tg:4 + 512 * tg + 512], pc(3), pc(4), ALU.mult, ALU.add, [("xpad", fc), (L, "lrup"), "cacc"], ["cacc"])
                        for jj in range(3):
                            o_ = cacc if jj < 2 else xcb[:, tk]
                            stt(o_, xpad[:, 1 + jj + 512 * tg:1 + jj + 512 * tg + 512], pc(jj), cacc, ALU.mult, ALU.add, [("xpad", fc), (L, "lrup"), "cacc"],
                                ["cacc"] if jj < 2 else [("xcb", fc, tg)])
                def lru_step(tg, fc):
                    xcb = xcb2[fc]; glg = glg2[fc]
                    pc = lambda i, fc=fc: lrup[:, fc, i:i + 1]
                    sl = fc
                    tk = slice(512 * tg, 512 * tg + 512)
                    hs = hs2[fc]
                    hsl = tg % 2
                    tp = tg % 2
                    ops = []
                    ops.append(lambda: mm(PSB[2 + sl][:, :], WAbd[:, fc, :], xcb[:, tk], True, True, [(L, "WAbd"), ("xcb", fc, tg)], [PK(2 + sl)]))
                    ops.append(lambda: mm(PSB[4 + sl][:, :], WXbd[:, fc, :], xcb[:, tk], True, True, [(L, "WAbd"), ("xcb", fc, tg)], [PK(4 + sl)]))
                    ops.append(lambda: act(rr[sl], PSB[2 + sl][:, :], AF.Sigmoid, [PK(2 + sl)], [("rr", sl)], bias=pc(5), scale=1.0))
                    ops.append(lambda: act(ii[sl], PSB[4 + sl][:, :], AF.Sigmoid, [PK(4 + sl)], [("ii", sl)], bias=pc(6), scale=1.0))
                    ops.append(lambda: act(aa[sl][tp], rr[sl], AF.Exp, [("rr", sl)], [("aa", sl, tp)], scale=pc(8)))
                    ops.append(lambda: act(a2[sl], rr[sl], AF.Exp, [("rr", sl)], [("a2", sl)], scale=pc(11)))
                    ops.append(lambda: ts(a2[sl], a2[sl], -1.0, 1.0, ALU.mult, ALU.add, [("a2", sl)], [("a2", sl)]))
                    ops.append(lambda: tt(ii[sl], ii[sl], xcb[:, tk], ALU.mult, [("ii", sl), ("xcb", fc, tg)], [("ii", sl)]))
                    ops.append(lambda: ts(a2[sl], a2[sl], 0.0, None, ALU.max, None, [("a2", sl)], [("a2", sl)]))
                    ops.append(lambda: act(a2[sl], a2[sl], AF.Sqrt, [("a2", sl)], [("a2", sl)]))
                    ops.append(lambda: tt(bb[sl][tp], ii[sl], a2[sl], ALU.mult, [("ii", sl), ("a2", sl)], [("bb", sl, tp)]))
                    init = 0.0 if tg == 0 else hs[1 - hsl][:, 511:512]
                    ops.append(lambda: P.op("dve", (lambda e, o=hs[hsl], d0=aa[sl][tp], d1=bb[sl][tp], init=init: e.tensor_tensor_scan(
                        out=o, data0=d0, data1=d1, initial=init, op0=ALU.mult, op1=ALU.add)),
                        [("aa", sl, tp), ("bb", sl, tp), ("hs", fc, 1 - hsl)], [("hs", fc, hsl)]))
                    ops.append(lambda: tt(hg_[sl], hs[hsl], glg[:, tk], ALU.mult, [("hs", fc, hsl), ("glg", fc)], [("hg", sl)]))
                    tb_ = 6 + sl

                    def trs():
                        for i4 in range(4):
                            tr(PSB[tb_][:, 128 * i4:128 * i4 + 128], hg_[sl][:, 128 * i4:128 * i4 + 128], identf, [("hg", sl), "identf"], [PK(tb_)])
                    ops.append(trs)
                    ops.append(lambda: cp("act", Ylru[:, 4 * tg:4 * tg + 4, 128 * fc:128 * fc + 128], resh(PSB[tb_][:, :], [4, 128]), [PK(tb_)], [("Ylru", tg)]))
                    return ops

                NF = 11
                steps = [(lru_step(tg, 0), lru_step(tg, 1)) for tg in range(4)]

                def lru_emit(tg, lo, hi):
                    for a_, b_ in zip(steps[tg][0][lo:hi], steps[tg][1][lo:hi]):
                        a_(); b_()

                lru_emit(0, 0, NF)
                for tg in range(4):
                    if tg + 1 < 4:
                        lru_emit(tg + 1, 0, NF)
                    lru_emit(tg, NF, None)
                A.release("lru")
                P.barrier()

                A.mark()
                Wqkv = [Wqkv0, [A.bf16(8, 256) for _ in range(3)]]
                w3 = Din["w_in"][l].rearrange("(kc p) n -> p kc n", p=128)

                def load_attn_w(hp_, rd=()):
                    for wi_ in range(3):
                        dma("pool", Wqkv[hp_][wi_], w3[:, :, 512 * wi_ + 256 * hp_:512 * wi_ + 256 * hp_ + 256], rd, [("Wqkv", hp_)])

                for hp in range(2):
                    A.mark()
                    qa = A.bf16(4, S)
                    ka = A.bf16(4, S)
                    Va = A.bf16(16, 4, 65)
                    Wq, Wk, Wv = Wqkv[hp]
                    pT = [A.bf16(512) for _ in range(4)]
                    gB = A.f32(16, 4, 8)
                    negc = A.f32(16, 8); gA = A.f32(16, 8)
                    kbias = A.f32(4, 16)
                    MBA = A.bf16(16, 4, 72)
                    gmA = A.f32(16, 4, 8); top8A = A.f32(16, 4, 8); t1A = A.f32(16, 4, 8)
                    km32 = A.f32(4, 8)
                    kmb = A.bf16(4, 8)
                    rec = [A.f32(1) for _ in range(4)]
                    dma("sp", gB, Din["gB"][hp], (), ["gB"])
                    dma("sp", negc, Din["negc"], (), ["negc"])
                    dma("sp", gA, Din["gA"], (), ["gA"])
                    dma("sp", kbias, Din["kbias"][:, 4 * hp:4 * hp + 4, :], (), ["kbias"])
                    for hh in range(4):
                        dma("sp", ka[64:72, hh, :], Din["blockind"], (), [("ka", hh)])
                    memset("dve", MBA.rearrange("p a b c -> p (a b c)"), 0.0, ["MBA"])
                    memset("dve", Va.rearrange("p a b c -> p (a b c)"), 1.0, ["Va"])
                    for tg in range(4):
                        tok = slice(512 * tg, 512 * tg + 512)
                        tkl = slice(512 * tg, 512 * tg + 512)
                        for hh in range(4):
                            for which, W, dst in ((0, Wq, qa), (1, Wk, ka)):
                                b = (2 * hh + which) % 2
                                for kc in range(8):
                                    mm(PSB[b][0:64, :], W[:, kc, 64 * hh:64 * hh + 64], hT[:, kc, tok], kc == 0, kc == 7,
                                       [("Wqkv", hp), ("hT", 0)], [PK(b)])
                                if which == 0:
                                    act(dst[0:64, hh, tkl], PSB[b][0:64, :], AF.Copy, [PK(b)], [("qa", hh)], scale=0.125)
                                else:
                                    cp("dve", dst[0:64, hh, tkl], PSB[b][0:64, :], [PK(b)], [("ka", hh)])

                    def emit_vproj():
                        for tti in range(16):
                            b = 2 + (tti % 2)
                            for kc in range(8):
                                mm(PSB[b][:, 0:256], hT[:, kc, 128 * tti:128 * tti + 128], Wv[:, kc, :], kc == 0, kc == 7,
                                   [("Wqkv", hp), ("hT", 0)], [PK(b)])
                            cp("act", Va[:, tti, :, 0:64], resh(PSB[b][:, 0:256], [4, 64]), [PK(b), "Va"], ["Va"])
                        if hp == 0:
                            load_attn_w(1, ["Va"])
                    for hh in range(4):
                        P.op("dve", (lambda e, hh=hh: e.tensor_reduce(out=km32[0:64, hh, :], in_=resh(ka[0:64, hh, :], [8, 256]),
                                                                         axis=AX.X, op=ALU.add)), [("ka", hh)], ["km32"])
                    ts(kmb[0:64].rearrange("p a b -> p (a b)"), km32[0:64].rearrange("p a b -> p (a b)"), 1.0 / 256.0, None, ALU.mult, None,
                       ["km32"], ["kmb"])
                    gb_ = 6
                    for tti in range(16):
                        for hh in range(4):
                            mm(PSB[gb_][:, 32 * tti + 8 * hh:32 * tti + 8 * hh + 8], qa[0:64, hh, 128 * tti:128 * tti + 128], kmb[0:64, hh, :], True, True,
                               [("qa", hh), "kmb"], [PK(gb_)])
                    emit_vproj()
                    tt(gmA, resh(PSB[gb_][:, :], [16, 4, 8]), negc.unsqueeze(2).to_broadcast([128, 16, 4, 8]), ALU.add, [PK(gb_), "negc"], ["gmA"])
                    for tti in range(16):
                        for hh in range(4):
                            P.op("dve", (lambda e, o=top8A[:, tti, hh, :], i_=gmA[:, tti, hh, :]: e.max(out=o, in_=i_)), ["gmA"], [("top8A", tti)])
                    tt(t1A, gmA, top8A[:, :, :, 2:3].to_broadcast([128, 16, 4, 8]), ALU.is_ge, ["gmA"] + [("top8A", t_) for t_ in range(16)], ["t1A"])
                    tt(t1A, t1A, gA.unsqueeze(2).to_broadcast([128, 16, 4, 8]), ALU.mult, ["t1A", "gA"], ["t1A"])
                    tt(MBA[:, :, :, 64:72], t1A, gB, ALU.add, ["t1A", "gB", "MBA"], ["MBA"])
                    for tti in range(16):
                        tb = 7 if tti % 2 == 0 else 5
                        tpv = resh(psb(tb, 512), [4, 128])
                        for hh in range(4):
                            tr(tpv[0:72, hh, :], MBA[:, tti, hh, :], identb, ["MBA", "identb"], [PK(tb)])
                        cp("act" if tti % 2 == 0 else "dve", qa[64:72, :, 128 * tti:128 * tti + 128], tpv[64:72, :, :], [PK(tb)], [("qa", 0), ("qa", 1), ("qa", 2), ("qa", 3)])
                    units = [(hh, g, kt) for hh in range(4) for g in range(4) for kt in range(4 * g + 4)]
                    SBK = (0, 1, 6, 7)

                    def emit_S(i):
                        hh, g, kt = units[i]
                        j = kt - 4 * g
                        qc0 = 128 * j if j > 0 else 0
                        ncol = 512 - qc0
                        sb_ = SBK[i % 4]
                        diag = j >= 0
                        mm(PSB[sb_][:, 0:ncol], ka[0:72, hh, 128 * kt:128 * kt + 128],
                           qa[0:72, hh, 512 * g + qc0:512 * g + 512], True, not diag, [("ka", hh), ("qa", hh)], [PK(sb_)])
                        if diag:
                            mm(PSB[sb_][:, 0:128], identb, trimask, False, True, ["identb", "trimask"], [PK(sb_)])

                    def emit_rest(i):
                        hh, g, kt = units[i]
                        hg = 4 * hp + hh
                        j = kt - 4 * g
                        qc0 = 128 * j if j > 0 else 0
                        ncol = 512 - qc0
                        sb_ = SBK[i % 4]
                        sl = i % 4
                        act(pT[sl][:, 0:ncol], PSB[sb_][:, 0:ncol], AF.Exp, [PK(sb_), "kbias"], [("pT", sl)],
                            bias=kbias[:, hh, j + 12:j + 13], scale=1.0)
                        for c in range(ncol // 128):
                            jq = (qc0 // 128) + c
                            ob = 2 + jq
                            mm(PSB[ob][:, 0:65], pT[sl][:, 128 * c:128 * c + 128], Va[:, kt, hh, :], kt == 0, kt == 4 * g + jq,
                               [("pT", sl), "Va"], [PK(ob)])
                            if kt == 4 * g + jq:
                                tti = 4 * g + jq
                                recip(rec[jq], PSB[ob][:, 64:65], [PK(ob)], [("rec", jq)])
                                P.op("dve", (lambda e, o=Yattn[:, tti, 64 * hg:64 * hg + 64], i_=PSB[ob][:, 0:64], s_=rec[jq]:
                                             e.tensor_scalar(out=o, in0=i_, scalar1=s_, scalar2=None, op0=ALU.mult)),
                                     [PK(ob), ("rec", jq)], [("Yattn", tti)])

                    LOOK = 2
                    for i in range(min(LOOK, len(units))):
                        emit_S(i)
                    for i in range(len(units)):
                        if i + LOOK < len(units):
                            emit_S(i + LOOK)
                        emit_rest(i)
                    A.release("attn")
                    if hp == 1:
                        P.barrier()
                A.release()
                if DEBUG and l == 0:
                    A.mark()
                    dt_ = A.f32(512)
                    for tti in range(16):
                        cp("dve", dt_, Yattn[:, tti, :], [("Yattn", tti)], ["dt_"])
                        dma("sp", dbg["ya"][T0 + 128 * tti:T0 + 128 * tti + 128, :], dt_, ["dt_"], [("dbgya", tti)])
                    A.release()
                    P.barrier()

                A.release()

                A.mark()
                Wo = A.bf16(8, 1024)
                lng = A.f32(1024); lnb = A.f32(1024)
                ln_tmp = [(A.f32(2, 6), A.f32(2), A.f32(1), A.f32(1), A.f32(1), A.f32(1024), A.f32(1024)) for _ in range(3)]
                ht = [A.f32(1024) for _ in range(2)]
                acc = [A.f32(1024) for _ in range(2)]
                yn = [A.bf16(1024) for _ in range(2)]
                ynT = [A.bf16(8, 128) for _ in range(2)]
                rs = [A.f32(8) for _ in range(2)]
                sqt = [A.f32(512) for _ in range(2)]
                dma("pool", Wo, Din["w_out"][l].rearrange("(kc p) n -> p kc n", p=128), (), ["Wo"])
                load_ln("ln1_g", "ln1_b", l)

                def op_f1(c):
                    sl = c % 2
                    k = lambda n: ("op", n, sl)
                    act(sqt[0], Yattn[:, c, :], AF.Square, [("Yattn", c), "sqt0"], ["sqt0", k("rs0")], accum=rs[sl][:, 0:1])
                    act(sqt[1][:, 0:256], Ylru[:, c, :], AF.Square, [("Ylru", c // 4), "sqt1"], ["sqt1", k("rs2")], accum=rs[sl][:, 2:3])
                    cp("dve", rs[sl][:, 1:2], ssqS[:, c:c + 1], [], [k("rs1")])
                    ts(rs[sl][:, 0:1], rs[sl][:, 0:1], 1.0 / 512, None, ALU.mult, None, ["sqt0", k("rs0")], [k("rs0")])
                    ts(rs[sl][:, 1:3], rs[sl][:, 1:3], 1.0 / 256, None, ALU.mult, None, ["sqt1", k("rs1"), k("rs2")], [k("rsm")])
                    act(rs[sl][:, 3:6], rs[sl][:, 0:3], AF.Sqrt, [k("rs0"), k("rsm")], [k("rsq")], bias=CRMS, scale=1.0)
                    recip(rs[sl][:, 3:6], rs[sl][:, 3:6], [k("rsq")], [k("rstd")])
                    stt(yn[sl][:, 0:512], Yattn[:, c, :], rs[sl][:, 3:4], mixg_b[:, 0:512], ALU.mult, ALU.mult,
                        [("Yattn", c), k("rstd"), (L, "mixgb")], [k("yn")])
                    stt(yn[sl][:, 512:768], Yssm[:, c, :], rs[sl][:, 4:5], mixg_b[:, 512:768], ALU.mult, ALU.mult,
                        [("Yssm", c), k("rstd"), (L, "mixgb")], [k("yn")])
                    stt(yn[sl][:, 768:1024], Ylru[:, c, :], rs[sl][:, 5:6], mixg_b[:, 768:1024], ALU.mult, ALU.mult,
                        [("Ylru", c // 4), k("rstd"), (L, "mixgb")], [k("yn")])

                def op_f2(c):
                    sl = c % 2
                    tok0 = T0 + 128 * c
                    k = lambda n: ("op", n, sl)
                    dma("sp", ht[sl], hbuf[tok0:tok0 + 128, :], [("hbuf", tok0)], [k("ht")])
                    tb = 6
                    tpv = resh(psb(tb, 1024), [8, 128])
                    for j in range(8):
                        tr(tpv[:, j, :], yn[sl][:, 128 * j:128 * j + 128], identb, [k("yn"), "identb"], [PK(tb)])
                    cp("act", ynT[sl], tpv, [PK(tb)], [k("ynT")])

                def op_f3(c):
                    sl = c % 2
                    k = lambda n: ("op", n, sl)
                    for nn in range(2):
                        ns = slice(512 * nn, 512 * nn + 512)
                        bnk = 2 * sl + nn
                        for j in range(8):
                            mm(PSB[bnk][:, :], ynT[sl][:, j, :], Wo[:, j, ns], j == 0, j == 7, [k("ynT"), "Wo"], [PK(bnk)])

                def op_back(c):
                    sl = c % 2
                    tok0 = T0 + 128 * c
                    k = lambda n: ("op", n, sl)
                    for nn in range(2):
                        ns = slice(512 * nn, 512 * nn + 512)
                        bnk = 2 * sl + nn
                        stt(acc[sl][:, ns], ht[sl][:, ns], ALPHA, PSB[bnk][:, :], ALU.mult, ALU.add, [k("ht"), PK(bnk)], [k("acc")])
                    return ln_tile(acc[sl], k("acc"), lng, lnb, tok0, c % 3, psbanks=(4, 5), dbgh=(l == 0), staged=True, geng="dve")

                def op_pre(c):
                    if c + 2 < 16:
                        op_f1(c + 2)
                    if c + 1 < 16:
                        op_f2(c + 1)
                    op_f3(c)

                op_f1(0); op_f1(1); op_f2(0)
                ln_pipeline(16, op_back, pre=op_pre)
                A.release("outproj")
                P.barrier()
                A.release()
            A.release()
            P.barrier()

            A.mark()
            LG = A.f32(16, 20)
            A.mark()
            kT = A.bf16(8, 512)
            Vm = A.bf16(4, 1024)
            Wq = A.bf16(8, 1024); Wo2 = A.bf16(8, 1024)
            A.mark()
            Wk = A.bf16(8, 1024); Wv = A.bf16(8, 1024)
            memT = A.bf16(8, 512)
            xm = [A.f32(1024) for _ in range(2)]
            for t in range(2 * SQ, 2 * SQ + 2):
                sl = t % 2
                dma("sp", xm[sl], Din["mem"][128 * t:128 * t + 128, :], (), [("xm", sl)])
                for half in range(2):
                    b = 6 + half
                    for c in range(4):
                        tr(PSB[b][:, 128 * c:128 * c + 128], xm[sl][:, 512 * half + 128 * c:512 * half + 128 * c + 128], identf,
                           [("xm", sl), "identf"], [PK(b)])
                    cp("act" if half == 0 else "dve", memT[:, 4 * half:4 * half + 4, 128 * t:128 * t + 128],
                       resh(psf(b), [4, 128]), [PK(b)], ["memT"])
            dma("pool", Wk, Din["mem_wk"][l].rearrange("(kc p) n -> p kc n", p=128), (), ["Wk"])
            dma("pool", Wv, Din["mem_wv"][l].rearrange("(kc p) n -> p kc n", p=128), (), ["Wv"])
            dma("pool", Wq, Din["mem_wq"][l].rearrange("(kc p) n -> p kc n", p=128), ["memT"], ["Wq"])
            dma("pool", Wo2, Din["mem_wo"][l].rearrange("(kc p) n -> p kc n", p=128), ["memT"], ["Wo2"])
            for fcn in range(8):
                b = fcn % 2
                ms_ = slice(256 * SQ, 256 * SQ + 256)
                for kc in range(8):
                    mm(PSB[b][:, 0:256], Wk[:, kc, 128 * fcn:128 * fcn + 128], memT[:, kc, ms_], kc == 0, kc == 7, ["Wk", "memT"], [PK(b)])
                cp("act" if b == 0 else "dve", kT[:, fcn, ms_], PSB[b][:, 0:256], [PK(b)], ["kT"])
            for t in range(2 * SQ, 2 * SQ + 2):
                for nn in range(2):
                    b = 2 + nn
                    for kc in range(8):
                        mm(PSB[b][:, :], memT[:, kc, 128 * t:128 * t + 128], Wv[:, kc, 512 * nn:512 * nn + 512], kc == 0, kc == 7, ["Wv", "memT"], [PK(b)])
                    cp("act" if nn == 0 else "dve", Vm[:, t, 512 * nn:512 * nn + 512], PSB[b][:, :], [PK(b)], ["Vm"])
            A.release("crossKV")
            P.barrier()
            lng = A.f32(1024); lnb = A.f32(1024)
            ln_tmp = [(A.f32(2, 6), A.f32(2), A.f32(1), A.f32(1), A.f32(1), A.f32(1024), A.f32(1024)) for _ in range(3)]
            ht = [A.f32(1024) for _ in range(2)]
            acc = [A.f32(1024) for _ in range(2)]
            qT2 = [A.bf16(8, 512) for _ in range(2)]
            PTm = [A.bf16(2, 512) for _ in range(2)]
            Rr = [A.f32(512) for _ in range(2)]
            oT2 = [A.bf16(8, 512) for _ in range(2)]
            wr = A.f32(8, 20)
            brb = A.f32(20)
            hTf = [A.f32(8, 128) for _ in range(2)]
            dma("sp", wr[:, :, 0:4], Din["moe_wr_g"][l].rearrange("(kc p) n -> p kc n", p=128), (), ["wr"])
            dma("sp", wr[:, :, 4:20], Din["moe_wr_e"][l].rearrange("(kc p) n -> p kc n", p=128), (), ["wr"])
            dma("sp", brb[:, 0:4], Din["moe_br_g"][l].partition_broadcast(128), (), ["brb"])
            dma("sp", brb[:, 4:20], Din["moe_br_e"][l].partition_broadcast(128), (), ["brb"])
            load_ln("ln2_g", "ln2_b", l)

            def emit_qT(g, fcns):
                tok = slice(512 * (g - 4 * SQ), 512 * (g - 4 * SQ) + 512)
                for fcn in fcns:
                    b = fcn % 2
                    for kc in range(8):
                        mm(PSB[b][:, :], Wq[:, kc, 128 * fcn:128 * fcn + 128], hT[:, kc, tok], kc == 0, kc == 7, ["Wq"], [PK(b)])
                    cp("act" if b == 0 else "dve", qT2[g % 2][:, fcn, :], PSB[b][:, :], [PK(b)], [("qT", g % 2, fcn)])

            def emit_scores(g, h):
                s = g // 4
                qT = qT2[g % 2]
                sl = h % 2
                for kt in range(2):
                    b = 2 + 2 * sl + kt
                    for dc in range(2):
                        mm(PSB[b][:, :], kT[:, 2 * h + dc, 256 * s + 128 * kt:256 * s + 128 * kt + 128], qT[:, 2 * h + dc, :], dc == 0, dc == 1,
                           ["kT", ("qT", g % 2, 2 * h), ("qT", g % 2, 2 * h + 1)], [PK(b)])
                    act(PTm[sl][:, kt, :], PSB[b][:, :], AF.Exp, [PK(b)], [("PTm", sl)], scale=1.0 / 16.0)

            def emit_pv(g, h):
                s = g // 4
                sl = h % 2
                for kt in range(2):
                    mm(PSB[6][:, :], onesb, PTm[sl][:, kt, :], kt == 0, kt == 1, [("PTm", sl), "onesb"], [PK(6)])
                recip(Rr[sl], PSB[6][:, :], [PK(6)], [("Rr", sl)])
                for dc in range(2):
                    b = 7 - dc
                    for kt in range(2):
                        mm(PSB[b][:, :], Vm[:, 2 * s + kt, 256 * h + 128 * dc:256 * h + 128 * dc + 128], PTm[sl][:, kt, :], kt == 0, kt == 1,
                           [("PTm", sl), "Vm"], [PK(b)])
                    tt(oT2[g % 2][:, 2 * h + dc, :], PSB[b][:, :], Rr[sl], ALU.mult, [PK(b), ("Rr", sl)], [("oT", g % 2, 2 * h + dc)])

            def tail_load(c):
                tok0 = 128 * c
                dma("act", ht[c % 2], hbuf[tok0:tok0 + 128, :], [("hbuf", tok0)], [("xo", "ht", c % 2)])

            def tail_mm(g, t4):
                oT_ = oT2[g % 2]
                for nn in range(2):
                    ns = slice(512 * nn, 512 * nn + 512)
                    for kc in range(8):
                        mm(PSB[nn][:, :], oT_[:, kc, 128 * t4:128 * t4 + 128], Wo2[:, kc, ns], kc == 0, kc == 7, [("oT", g % 2, kc), "Wo2"], [PK(nn)])

            def tail_back(g, t4):
                c = 4 * g + t4
                sl = c % 2
                k = lambda n: ("xo", n, sl)
                for nn in range(2):
                    ns = slice(512 * nn, 512 * nn + 512)
                    stt(acc[sl][:, ns], ht[sl][:, ns], ALPHA, PSB[nn][:, :], ALU.mult, ALU.add, [k("ht"), PK(nn)], [k("acc")])
                stg_ = ln_tile(acc[sl], k("acc"), lng, lnb, 128 * c, c % 3, psbanks=(0, 1), staged=True, tcopy=(hTf[sl], ("hTf", sl)), geng="dve")

                def logits():
                    for kc in range(8):
                        mm(PSB[1][:, 0:20], hTf[sl][:, kc, :], wr[:, kc, :], kc == 0, kc == 7, [("hTf", sl), "wr"], [PK(1)])
                    tt(LG[:, c - 16 * SQ, :], PSB[1][:, 0:20], brb, ALU.add, [PK(1), "brb"], [("LG", c)])

                return stg_ + [logits]

            g0 = 4 * SQ
            pstg = []

            def pipe_step(make):
                it = len(pstg)
                pstg.append(make() if make is not None else None)
                if pstg[it] is not None:
                    pstg[it][0]()
                for kk in (1, 2, 4, 3):
                    j = it - kk
                    if j >= 0 and pstg[j] is not None:
                        pstg[j][kk]()

            def tail_step(g, t4):
                c = 4 * g + t4
                if c + 1 < 4 * g0 + 16:
                    tail_load(c + 1)
                tail_mm(g, t4)
                pipe_step(lambda: tail_back(g, t4))

            emit_qT(g0, range(8))
            tail_load(4 * g0)
            for g in range(g0, g0 + 4):
                emit_scores(g, 0)
                for h in range(4):
                    if h + 1 < 4:
                        emit_scores(g, h + 1)
                    if g + 1 < g0 + 4:
                        emit_qT(g + 1, (2 * h, 2 * h + 1))
                    emit_pv(g, h)
                    if g > g0:
                        tail_step(g - 1, h)
            for t4 in range(4):
                tail_step(g0 + 3, t4)
            for _ in range(4):
                pipe_step(None)
            A.release("cross")
            P.barrier()

            A.mark()
            combT = A.bf16(NTOK)
            selE = A.bf16(16, 128)
            Wgu = [A.bf16(8, 512) for _ in range(2)]
            Wd = [A.bf16(2, 1024) for _ in range(2)]

            def load_expert(e, ws):
                dma("pool", Wgu[ws][:, :, 0:256], Din["moe_w_gate"][l][e].rearrange("(kc p) n -> p kc n", p=128), (), [("Wgu", ws)])
                dma("pool", Wgu[ws][:, :, 256:512], Din["moe_w_up"][l][e].rearrange("(kc p) n -> p kc n", p=128), (), [("Wgu", ws)])
                dma("pool", Wd[ws], Din["moe_w_down"][l][e].rearrange("(kc p) n -> p kc n", p=128), (), [("Wd", ws)])

            load_expert(0, 0)
            load_expert(1, 1)
            A.mark()
            NT = 16
            R1 = [A.f32(NT) for _ in range(10)]
            R4 = [A.f32(NT, 4) for _ in range(8)]
            R16 = [A.f32(NT, 16) for _ in range(2)]
            cp("dve", selE[0:16], identf[0:16, 0:16].unsqueeze(2).to_broadcast([16, 16, 128]), ["identf"], ["selE"])
            rk = ["rtb"]
            gl_ = LG[:, :, 0:4]
            el_ = LG[:, :, 4:20].rearrange("p c (g e) -> p c g e", g=4)
            gmax, ngm, gsum, gw, m1_, m2_, dlt, w2_, w1_ = R1[0:9]
            goh, gex, eig, oh1, eig2, oh2, loc, tmp4 = R4
            em, comb = R16
            b3 = lambda x: x.unsqueeze(2).to_broadcast([128, NT, 4])
            P.op("dve", lambda e: e.tensor_reduce(out=gmax, in_=gl_, axis=AX.X, op=ALU.max), ["LG"] + rk, rk)
            tt(goh, gl_, b3(gmax), ALU.is_equal, ["LG"] + rk, rk)
            tt(gex, gl_, b3(gmax), ALU.subtract, ["LG"] + rk, rk)
            act(gex, gex, AF.Exp, rk, rk)
            P.op("dve", lambda e: e.tensor_reduce(out=gsum, in_=gex, axis=AX.X, op=ALU.add), rk, rk)
            recip(gw, gsum, rk, rk)
            em4 = em.rearrange("p c (g e) -> p c g e", g=4)
            tt(em4, el_, goh.unsqueeze(3).to_broadcast([128, NT, 4, 4]), ALU.mult, ["LG"] + rk, rk)
            P.op("dve", lambda e: e.tensor_reduce(out=eig, in_=em.rearrange("p c (g e) -> p c e g", g=4), axis=AX.X, op=ALU.add), rk, rk)
            P.op("dve", lambda e: e.tensor_reduce(out=m1_, in_=eig, axis=AX.X, op=ALU.max), rk, rk)
            tt(oh1, eig, b3(m1_), ALU.is_equal, rk, rk)
            stt(eig2, oh1, -1e30, eig, ALU.mult, ALU.add, rk, rk)
            P.op("dve", lambda e: e.tensor_reduce(out=m2_, in_=eig2, axis=AX.X, op=ALU.max), rk, rk)
            tt(oh2, eig2, b3(m2_), ALU.is_equal, rk, rk)
            tt(dlt, m2_, m1_, ALU.subtract, rk, rk)
            act(w2_, dlt, AF.Sigmoid, rk, rk)
            ts(w1_, w2_, -1.0, 1.0, ALU.mult, ALU.add, rk, rk)
            tt(loc, oh1, b3(w1_), ALU.mult, rk, rk)
            tt(tmp4, oh2, b3(w2_), ALU.mult, rk, rk)
            tt(loc, loc, tmp4, ALU.add, rk, rk)
            tt(loc, loc, b3(gw), ALU.mult, rk, rk)
            comb4 = comb.rearrange("p c (g e) -> p c g e", g=4)
            tt(comb4, goh.unsqueeze(3).to_broadcast([128, NT, 4, 4]), loc.unsqueeze(2).to_broadcast([128, NT, 4, 4]), ALU.mult, rk, rk)
            for c4 in range(4):
                tb = 4 + (c4 % 2)
                for t in range(4):
                    c = 4 * c4 + t
                    tr(PSB[tb][0:16, 128 * t:128 * t + 128], comb[:, c, :], identf, rk + ["identf"], [PK(tb)])
                cp("act", combT[0:16, T0 + 512 * c4:T0 + 512 * c4 + 512], PSB[tb][0:16, :], [PK(tb)], ["combT"])
            A.release("router")
            P.barrier()
            lng = A.f32(1024); lnb = A.f32(1024)
            ln_tmp = [(A.f32(2, 6), A.f32(2), A.f32(1), A.f32(1), A.f32(1), A.f32(1024), A.f32(1024)) for _ in range(3)]
            ht = [A.f32(1024) for _ in range(2)]
            TGS = 1024
            accG2 = [A.f32(TGS // 128, 1024) for _ in range(2)]
            HEp = [A.bf16(2, 2, TGS) for _ in range(2)]
            sgt = [A.f32(512) for _ in range(2)]
            upt = [A.f32(512) for _ in range(2)]
            load_ln("ln3_g", "ln3_b", l)
            drot = 0

            def moe_load(G, t8):
                c = (TGS * G) // 128 + t8
                tok0 = 128 * c
                dma("act", ht[c % 2], hbuf[tok0:tok0 + 128, :], [("hbuf", tok0)], [("mo_ht", c % 2)])

            def moe_tail(G, t8):
                accG = accG2[G % 2]
                c = (TGS * G) // 128 + t8
                sl = c % 2
                tok0 = 128 * c
                stt(accG[:, t8, :], ht[sl], ALPHA, accG[:, t8, :], ALU.mult, ALU.add, [("mo_ht", sl), ("accG", G % 2, t8)], [("accG", G % 2, t8)])
                return ln_tile(accG[:, t8, :], ("accG", G % 2, t8), lng, lnb, tok0, c % 3, final=(l == 1), psbanks=(0, 1), staged=True, geng="dve")

            G0 = 2 * SQ
            for G in range(G0, G0 + 2):
                accG = accG2[G % 2]
                for ep in range(8):
                    hb = ep % 2
                    micro = []
                    if G > G0:
                        if ep == 0:
                            moe_load(G - 1, 0)
                        if ep + 1 < 8:
                            moe_load(G - 1, ep + 1)
                        micro = [lambda G=G, ep=ep: micro.extend(moe_tail(G - 1, ep))]
                    for ei in range(2):
                        e = 2 * ep + ei
                        ws = ei
                        if not (G == G0 and ep == 0):
                            load_expert(e, ws)
                        for q5 in range(TGS // 512):
                            tok = slice(TGS * G + 512 * q5, TGS * G + 512 * q5 + 512)
                            tokl = slice(TGS * G + 512 * q5 - T0, TGS * G + 512 * q5 + 512 - T0)
                            mm(PSB[6][:, :], selE[0:16, e, :], combT[0:16, tok], True, True, ["selE", "combT"], [PK(6)])
                            for dc in range(2):
                                sl = dc
                                for kc in range(8):
                                    mm(PSB[2 * dc][:, :], Wgu[ws][:, kc, 128 * dc:128 * dc + 128], hT[:, kc, tokl], kc == 0, kc == 7, [("Wgu", ws)], [PK(2 * dc)])
                                for kc in range(8):
                                    mm(PSB[2 * dc + 1][:, :], Wgu[ws][:, kc, 256 + 128 * dc:256 + 128 * dc + 128], hT[:, kc, tokl], kc == 0, kc == 7, [("Wgu", ws)], [PK(2 * dc + 1)])
                                act(sgt[sl], PSB[2 * dc][:, :], AF.Silu, [PK(2 * dc)], [("sgt", sl)])
                                tt(upt[sl], PSB[2 * dc + 1][:, :], sgt[sl], ALU.mult, [PK(2 * dc + 1), ("sgt", sl)], [("upt", sl)])
                                tt(HEp[hb][:, ei, dc, 512 * q5:512 * q5 + 512], PSB[6][:, :], upt[sl], ALU.mult, [PK(6), ("upt", sl)], [("HE", hb, ei)])
                            if micro:
                                if ei == 0 and q5 == 0:
                                    micro.pop(0)()
                                    micro.pop(0)()
                                elif len(micro) > 1:
                                    micro.pop(0)()
                    if micro:
                        while len(micro) > 1:
                            micro.pop(0)()
                        pb_ = ((4, 5, 7)[drot % 3], (4, 5, 7)[(drot + 1) % 3])
                        drot += 2
                        micro.pop(0)(pb_)
                    if G == G0 + 1 and ep == 7:
                        moe_load(G0 + 1, 0)
                    for t8 in range(TGS // 128):
                        for nn in range(2):
                            ns = slice(512 * nn, 512 * nn + 512)
                            b = (4, 5, 7)[drot % 3]
                            drot += 1
                            for ei in range(2):
                                for dc in range(2):
                                    mm(PSB[b][:, :], HEp[hb][:, ei, dc, 128 * t8:128 * t8 + 128], Wd[ei][:, dc, ns], ei == 0 and dc == 0, ei == 1 and dc == 1,
                                       [("HE", hb, 0), ("HE", hb, 1), ("Wd", 0), ("Wd", 1)], [PK(b)])
                            if ep == 0:
                                cp("act", accG[:, t8, ns], PSB[b][:, :], [PK(b), ("accG", G % 2, t8)], [("accG", G % 2, t8)])
                            else:
                                tt(accG[:, t8, ns], PSB[b][:, :], accG[:, t8, ns], ALU.add, [PK(b), ("accG", G % 2, t8)], [("accG", G % 2, t8)])

            def drain_pre(i):
                if i + 1 < TGS // 128:
                    moe_load(G0 + 1, i + 1)

            ln_pipeline(TGS // 128, lambda i: moe_tail(G0 + 1, i), pre=drain_pre)
            A.release("moe")
            A.release()
            P.barrier()

    P.emit(st)
    st.close()
    return nc


_CACHE = {}


def kernel(**inputs):
    if "nc" not in _CACHE:
        _CACHE["nc"] = build_program()
    nc = _CACHE["nc"]
    consts = host_consts()
    x = np.ascontiguousarray(inputs["x"], dtype=np.float32)
    mem = np.ascontiguousarray(inputs["mem"], dtype=np.float32)
    in_maps = []
    for c in range(NCORES):
        m = {"x": x[2 * c:2 * c + 2].reshape(NTOK, D), "mem": mem[2 * c:2 * c + 2].reshape(512, D)}
        for k in PARAM_SHAPES:
            m[k] = np.ascontiguousarray(inputs[k], dtype=np.float32)
        for k, v in consts.items():
            m[k] = v
        in_maps.append(m)
    res = run_bass_kernel_spmd(nc, in_maps, core_ids=list(range(NCORES)))
    outs = [np.asarray(r["out"]).reshape(2, S, D) for r in res.results]
    full = np.concatenate(outs, axis=0).astype(np.float32)
    if DEBUG:
        kernel.dbg = [{k: np.asarray(v) for k, v in r.items() if k.startswith("dbg_")} for r in res.results]
    return full
```

```python
import math
from contextlib import ExitStack
import numpy as np
import ml_dtypes
import concourse.bass as bass
import concourse.mybir as mybir
from concourse.bass_utils import run_bass_kernel_spmd

F32 = mybir.dt.float32
BF16 = mybir.dt.bfloat16
I32 = mybir.dt.int32
AF = mybir.ActivationFunctionType
ALU = mybir.AluOpType
AX = mybir.AxisListType

DEBUG = False
ARENA_LOG = None
STOP_AFTER = None

NCORES = 8
S = 2048
NTOK = 2 * S
D = 1024
ALPHA = (2.0 * 2) ** 0.25
LN_EPS = 1e-5
RMS_EPS = 1e-6
NEGM = -30000.0
TWO_PI = 2.0 * math.pi


class Prog:
    ENGS = ("pe", "act", "dve", "pool", "sp")
    NDMA = 24

    def __init__(self, nc):
        self.nc = nc
        self.ins = []
        self.lastw = {}
        self.readers = {}
        self.bar = None
        self._bar_start = 0

    def _add(self, eng, fn, reads, writes, dma):
        idx = len(self.ins)
        deps = set()
        if self.bar is not None:
            deps.update(self.bar)
        for r in reads:
            w = self.lastw.get(r)
            if w is not None:
                deps.add(w)
        for w_ in writes:
            w = self.lastw.get(w_)
            if w is not None:
                deps.add(w)
            for rd in self.readers.get(w_, ()):
                deps.add(rd)
        deps.discard(idx)
        self.ins.append(dict(eng=eng, fn=fn, deps=deps, dma=dma, idx=idx))
        for r in reads:
            self.readers.setdefault(r, []).append(idx)
        for w_ in writes:
            self.lastw[w_] = idx
            self.readers[w_] = []
        return idx

    def op(self, eng, fn, reads=(), writes=()):
        return self._add(eng, fn, tuple(reads), tuple(writes), False)

    def dma(self, eng, fn, reads=(), writes=()):
        return self._add(eng, fn, tuple(reads), tuple(writes), True)

    def barrier(self):
        last = {}
        dmas = []
        for i in self.ins[self._bar_start:]:
            if i["dma"]:
                dmas.append(i["idx"])
            else:
                last[i["eng"]] = i["idx"]
        nb = set(last.values()) | set(dmas)
        if self.bar is not None:
            engs_new = {self.ins[j]["eng"] for j in last.values()}
            for j in self.bar:
                if self.ins[j]["dma"] or self.ins[j]["eng"] not in engs_new:
                    if not self.ins[j]["dma"]:
                        nb.add(j)
        self.bar = nb
        self._bar_start = len(self.ins)
        self.lastw = {}
        self.readers = {}

    def emit(self, stack):
        nc = self.nc
        ins = self.ins
        for i in ins:
            nd = set()
            for d in i["deps"]:
                p = ins[d]
                if (not p["dma"]) and (not i["dma"]) and p["eng"] == "pe" and i["eng"] == "pe":
                    continue
                nd.add(d)
            i["deps"] = nd
        needed = set()
        for i in ins:
            needed.update(i["deps"])
        esem = {e: stack.enter_context(nc.semaphore("s_" + e)) for e in self.ENGS}
        dsem = [stack.enter_context(nc.semaphore("d_%d" % k)) for k in range(self.NDMA)]
        cnt = {e: 0 for e in self.ENGS}
        dcnt = [0] * self.NDMA
        nd_ = 0
        for i in ins:
            if i["dma"]:
                k = nd_ % self.NDMA
                nd_ += 1
                i["prev"] = (dsem[k], dcnt[k]) if dcnt[k] > 0 else None
                dcnt[k] += 16
                i["sem"] = dsem[k]
                i["val"] = dcnt[k]
                i["semkey"] = ("d", k)
                i["signal"] = True
            else:
                if i["idx"] in needed:
                    cnt[i["eng"]] += 1
                    i["signal"] = True
                else:
                    i["signal"] = False
                i["sem"] = esem[i["eng"]]
                i["val"] = cnt[i["eng"]]
                i["semkey"] = ("e", i["eng"])
        per = {e: [i for i in ins if i["eng"] == e] for e in self.ENGS}
        block = stack.enter_context(nc.Block())

        def run(eng_obj, lst):
            waited = {}
            for i in lst:
                waits = {}
                for d in i["deps"]:
                    p = ins[d]
                    k = p["semkey"]
                    if waits.get(k, (None, 0))[1] < p["val"]:
                        waits[k] = (p["sem"], p["val"])
                if i["dma"] and i["prev"] is not None:
                    k = i["semkey"]
                    if waits.get(k, (None, 0))[1] < i["prev"][1]:
                        waits[k] = i["prev"]
                for k, (s, v) in waits.items():
                    if waited.get(k, 0) >= v:
                        continue
                    eng_obj.wait_ge(s, v)
                    waited[k] = v
                r = i["fn"](eng_obj)
                if i["dma"]:
                    r.then_inc(i["sem"], 16)
                elif i["signal"]:
                    r.then_inc(i["sem"], 1)
            return waited

        fin = {}
        for i in ins:
            if i["dma"]:
                fin[i["semkey"]] = (i["sem"], i["val"])

        @block.tensor
        def _(e):
            run(e, per["pe"])

        @block.scalar
        def _(e):
            run(e, per["act"])

        @block.vector
        def _(e):
            run(e, per["dve"])

        @block.gpsimd
        def _(e):
            waited = run(e, per["pool"])

        @block.sync
        def _(e):
            waited = run(e, per["sp"])
            for k, (s, v) in fin.items():
                if waited.get(k, 0) < v:
                    e.wait_ge(s, v)


def resh(ap, shape):
    shape = list(shape)
    if len(shape) == 1:
        return ap
    names = "abcd"[: len(shape)]
    kw = {n: v for n, v in zip(names[:-1], shape[:-1])}
    return ap.rearrange("p (%s) -> p %s" % (" ".join(names), " ".join(names)), **kw)


def host_consts():
    c = {}
    c["identf"] = np.eye(128, dtype=np.float32)
    s_ = np.arange(128)
    c["tri_cs"] = (s_[:, None] <= s_[None, :]).astype(np.float32)
    c["trimask"] = np.where(s_[:, None] <= s_[None, :], 0.0, NEGM).astype(np.float32)
    c["iota_row"] = np.tile(np.arange(128, dtype=np.float32)[None, :], (128, 1))
    c["iota_col"] = np.arange(128, dtype=np.float32)[:, None].copy()
    slopes = np.array([2.0 ** (-8.0 * (h + 1) / 8) for h in range(8)], np.float64)
    negc = np.zeros((128, 16, 8), np.float32)
    A = np.zeros((128, 16, 8), np.float32)
    Bc = np.zeros((128, 16, 8, 8), np.float32)
    for tt in range(16):
        own = tt // 2
        for n in range(8):
            negc[:, tt, n] = -1e30 if n >= own else 0.0
            A[:, tt, n] = 30000.0 if n < own else 0.0
        for h in range(8):
            cq = slopes[h] * (128 * (tt % 4) + np.arange(128))
            for n in range(8):
                Bc[:, tt, h, n] = (30000.0 if n == own else 0.0) - 30000.0 - cq
    c["negc"] = negc
    c["gA"] = A
    c["gB"] = np.ascontiguousarray(Bc.reshape(128, 16, 2, 4, 8).transpose(2, 0, 1, 3, 4))
    kb = np.zeros((128, 8, 16), np.float32)
    for h in range(8):
        for dd in range(16):
            kb[:, h, dd] = slopes[h] * (128 * (dd - 12) + np.arange(128))
    c["kbias"] = kb
    bi = np.zeros((8, 2048), np.float32)
    for n in range(8):
        bi[n, 256 * n:256 * (n + 1)] = 1.0
    c["blockind"] = bi.astype(ml_dtypes.bfloat16)
    cc = np.zeros((128, 8), np.float32)
    cc[:, 1] = 1.0
    cc[:, 2] = LN_EPS
    cc[:, 3] = RMS_EPS
    cc[:, 4] = math.pi / 2
    c["ccols"] = cc
    return c


CONST_SHAPES = {
    "identf": ([128, 128], F32), "tri_cs": ([128, 128], F32), "trimask": ([128, 128], F32),
    "iota_row": ([128, 128], F32), "iota_col": ([128, 1], F32), "negc": ([128, 16, 8], F32),
    "gA": ([128, 16, 8], F32), "gB": ([2, 128, 16, 4, 8], F32), "kbias": ([128, 8, 16], F32),
    "blockind": ([8, 2048], BF16), "ccols": ([128, 8], F32),
}

PARAM_SHAPES = {
    "ln0_g": [1024], "ln0_b": [1024], "w_in": [2, 1024, 2304], "mix_g": [2, 1024], "w_out": [2, 1024, 1024],
    "ssm_a_re": [2, 16, 64], "ssm_a_im": [2, 16, 64], "ssm_b_re": [2, 16, 64, 16], "ssm_b_im": [2, 16, 64, 16],
    "ssm_c_re": [2, 16, 16, 64], "ssm_c_im": [2, 16, 16, 64], "ssm_d": [2, 256], "ssm_log_dt": [2, 16],
    "ssm_w_glu": [2, 256, 256], "ssm_b_glu": [2, 256], "lru_conv_w": [2, 4, 256], "lru_conv_b": [2, 256],
    "lru_w_a": [2, 4, 64, 64], "lru_b_a": [2, 256], "lru_w_x": [2, 4, 64, 64], "lru_b_x": [2, 256],
    "lru_lam": [2, 256], "ln1_g": [2, 1024], "ln1_b": [2, 1024], "mem_wq": [2, 1024, 1024],
    "mem_wk": [2, 1024, 1024], "mem_wv": [2, 1024, 1024], "mem_wo": [2, 1024, 1024], "ln2_g": [2, 1024],
    "ln2_b": [2, 1024], "moe_wr_g": [2, 1024, 4], "moe_br_g": [2, 4], "moe_wr_e": [2, 1024, 16],
    "moe_br_e": [2, 16], "moe_w_gate": [2, 16, 1024, 256], "moe_w_up": [2, 16, 1024, 256],
    "moe_w_down": [2, 16, 256, 1024], "ln3_g": [2, 1024], "ln3_b": [2, 1024],
}


def build_program():
    nc = bass.Bass("TRN2", target_bir_lowering=False)
    Din = {}
    Din["x"] = nc.dram_tensor("x", [NTOK, D], F32, kind="ExternalInput").ap()
    Din["mem"] = nc.dram_tensor("mem", [512, D], F32, kind="ExternalInput").ap()
    for k, shp in PARAM_SHAPES.items():
        Din[k] = nc.dram_tensor(k, shp, F32, kind="ExternalInput").ap()
    for k, (shp, dt) in CONST_SHAPES.items():
        Din[k] = nc.dram_tensor(k, shp, dt, kind="ExternalInput").ap()
    out = nc.dram_tensor("out", [NTOK, D], F32, kind="ExternalOutput").ap()
    hbuf = nc.dram_tensor("hbuf", [NTOK, D], F32, kind="Internal").ap()
    PREPW = 10240
    prepbuf = nc.dram_tensor("prepbuf", [2, 128, PREPW], F32, kind="Internal").ap()
    dbg = {}
    if DEBUG:
        dbg["ya"] = nc.dram_tensor("dbg_ya", [NTOK, 512], F32, kind="ExternalOutput").ap()
        dbg["ys"] = nc.dram_tensor("dbg_ys", [NTOK, 256], F32, kind="ExternalOutput").ap()
        dbg["yl"] = nc.dram_tensor("dbg_yl", [256, NTOK], F32, kind="ExternalOutput").ap()
        dbg["h"] = nc.dram_tensor("dbg_h", [NTOK, D], F32, kind="ExternalOutput").ap()

    P = Prog(nc)
    st = ExitStack()
    SBW = 51200
    SBT = st.enter_context(nc.sbuf_tensor("SB", [128, SBW], F32))
    PSB = [st.enter_context(nc.psum_tensor("ps%d" % i, [128, 512], F32)) for i in range(8)]

    class Arena:
        def __init__(self):
            self.off = 0
            self.marks = []

        def f32(self, *shape):
            n = int(np.prod(shape))
            assert self.off + n <= SBW, ("SBUF overflow", self.off, n)
            ap = SBT[:, self.off:self.off + n]
            self.off += n
            return resh(ap, shape)

        def bf16(self, *shape):
            n = int(np.prod(shape))
            n32 = (n + 1) // 2
            assert self.off + n32 <= SBW, ("SBUF overflow", self.off, n32)
            ap = SBT[:, self.off:self.off + n32].bitcast(BF16)[:, 0:n]
            self.off += n32
            return resh(ap, shape)

        def i32(self, *shape):
            n = int(np.prod(shape))
            ap = SBT[:, self.off:self.off + n].bitcast(I32)
            self.off += n
            return resh(ap, shape)

        def mark(self):
            self.marks.append(self.off)

        def release(self, tag=None):
            if tag is not None and ARENA_LOG is not None:
                ARENA_LOG.append((tag, self.off * 4 // 1024))
            self.off = self.marks.pop()

    A = Arena()

    def psf(i, n=512):
        return PSB[i][:, 0:n]

    def psb(i, n=512):
        return PSB[i][:, 0:(n + 1) // 2].bitcast(BF16)[:, 0:n]

    def PK(i):
        return ("ps", i)

    def mm(o, lhsT, rhs, start, stop, r, w):
        P.op("pe", lambda e: e.matmul(o, lhsT=lhsT, rhs=rhs, start=start, stop=stop, skip_group_check=True), r, w)

    def tr(o, in_, ident, r, w):
        P.op("pe", lambda e: e.transpose(o, in_, ident), r, w)

    def act(o, in_, func, r, w, bias=None, scale=None, accum=None):
        kw = {}
        if bias is not None:
            kw["bias"] = bias
        if scale is not None:
            kw["scale"] = scale
        if accum is not None:
            kw["accum_out"] = accum
        P.op("act", lambda e: e.activation(out=o, in_=in_, func=func, **kw), r, w)

    def tt(o, a, b, op, r, w, eng="dve"):
        P.op(eng, lambda e: e.tensor_tensor(out=o, in0=a, in1=b, op=op), r, w)

    def ts(o, a, s1, s2, op0, op1, r, w, eng="dve", accum=None):
        if op1 is None:
            P.op(eng, lambda e: e.tensor_scalar(out=o, in0=a, scalar1=s1, scalar2=None, op0=op0), r, w)
        elif accum is not None:
            P.op(eng, lambda e: e.tensor_scalar(out=o, in0=a, scalar1=s1, scalar2=s2, op0=op0, op1=op1, accum_out=accum), r, w)
        else:
            P.op(eng, lambda e: e.tensor_scalar(out=o, in0=a, scalar1=s1, scalar2=s2, op0=op0, op1=op1), r, w)

    def stt(o, a, sc, b, op0, op1, r, w):
        P.op("dve", lambda e: e.scalar_tensor_tensor(out=o, in0=a, scalar=sc, in1=b, op0=op0, op1=op1), r, w)

    def cp(eng, o, in_, r, w):
        if eng == "act":
            P.op("act", lambda e: e.copy(out=o, in_=in_), r, w)
        else:
            P.op(eng, lambda e: e.tensor_copy(out=o, in_=in_), r, w)

    def memset(eng, o, val, w):
        P.op(eng, lambda e: e.memset(o, val), (), w)

    def dma(q, o, in_, r, w, slow=False, maxlast=None):
        kw = {}
        if slow:
            kw["allow_slow_non_contiguous"] = True
        if maxlast is not None:
            kw["max_dma_last_dim"] = maxlast
        P.dma(q, lambda e: e.dma_start(out=o, in_=in_, **kw), r, w)

    def recip(o, in_, r, w):
        P.op("dve", lambda e: e.reciprocal(out=o, in_=in_), r, w)

    hT = A.bf16(8, S)
    CUR = {"T0": 0}
    identf = A.f32(128)
    identb = A.bf16(128)
    tri_cs = A.bf16(128)
    trimask = A.bf16(128)
    onesb = A.bf16(128)
    iota_row = A.f32(128)
    iota_col = A.f32(1)
    ccols = A.f32(8)
    tmpc = A.f32(128)
    C0, C1, CEPS, CRMS, CHPI = (ccols[:, i:i + 1] for i in range(5))

    dma("sp", identf, Din["identf"], (), ["identf"])
    dma("sp", tmpc, Din["tri_cs"], (), ["tmpc"])
    cp("dve", tri_cs, tmpc, ["tmpc"], ["tri_cs"])
    cp("dve", identb, identf, ["identf"], ["identb"])
    dma("sp", tmpc, Din["trimask"], ["tmpc"], ["tmpc"])
    cp("dve", trimask, tmpc, ["tmpc"], ["trimask"])
    dma("sp", iota_row, Din["iota_row"], (), ["iota_row"])
    dma("sp", iota_col, Din["iota_col"], (), ["iota_col"])
    dma("sp", ccols, Din["ccols"], (), ["ccols"])
    memset("dve", onesb, 1.0, ["onesb"])
    P.barrier()

    def ln_tile(acc, kacc, gt, bt, tok0, slot, final=False, psbanks=(6, 7), dbgh=False, defer=False, geng="pool", staged=False, tcopy=None, beng=None):
        st6, mv, sd, rstd, nmr, yt, y2 = ln_tmp[slot]
        k = lambda n: ("ln", n, slot)
        tl_ = tok0 - CUR["T0"]
        kyt = k("yt")

        def s_a():
            P.op("dve", lambda e: e.bn_stats(out=st6[:, 0, :], in_=acc[:, 0:512]), [kacc], [k("st0")])
            P.op("dve", lambda e: e.bn_stats(out=st6[:, 1, :], in_=acc[:, 512:1024]), [kacc], [k("st1")])
            P.op("dve", lambda e: e.bn_aggr(out=mv, in_=st6.rearrange("p a b -> p (a b)")), [k("st0"), k("st1")], [k("mv")])
            act(sd, mv[:, 1:2], AF.Sqrt, [k("mv")], [k("sd")], bias=CEPS, scale=1.0)

        def s_b():
            recip(rstd, sd, [k("sd")], [k("rstd")])
            stt(nmr, mv[:, 0:1], -1.0, rstd, ALU.mult, ALU.mult, [k("mv"), k("rstd")], [k("nmr")])
            act(yt, acc, AF.Identity, [kacc, k("rstd"), k("nmr")], [k("yt")], bias=nmr, scale=rstd)

        def s_c():
            tt(y2, yt, gt, ALU.mult, [k("yt"), "lng"], [k("y2")], eng=geng)
            tt(yt, y2, bt, ALU.add, [k("y2"), "lnb"], [k("yt")], eng=(geng if beng is None else beng))
            if final:
                dma("sp", out[tok0:tok0 + 128, :], yt, [k("yt")], [("out", tok0)])
                return
            dma("sp", hbuf[tok0:tok0 + 128, :], yt, [k("yt")], [("hbuf", tok0)])
            if dbgh and DEBUG:
                dma("sp", dbg["h"][tok0:tok0 + 128, :], yt, [k("yt")], [("dbgh", tok0)])

        def emit_tr(pb=None):
            if final:
                return
            banks = psbanks if pb is None else pb
            for half in range(2):
                b = banks[half]
                for c in range(4):
                    tr(PSB[b][:, 128 * c:128 * c + 128], yt[:, 512 * half + 128 * c: 512 * half + 128 * c + 128], identf,
                       [kyt, "identf"], [PK(b)])
                o = hT[:, 4 * half:4 * half + 4, tl_:tl_ + 128]
                cp("act" if half == 0 else "dve", o, resh(psf(b), [4, 128]), [PK(b)], [("hT", tok0 // 128), ("psrd", b)])
                if tcopy is not None:
                    cp("dve" if half == 0 else "act", tcopy[0][:, 4 * half:4 * half + 4, :], resh(psf(b), [4, 128]),
                       [PK(b), ("psrd", b), tcopy[1]], [tcopy[1]])

        if staged:
            return [s_a, s_b, s_c, emit_tr]
        s_a(); s_b(); s_c()
        if final:
            return (lambda: None)
        if defer:
            return emit_tr
        emit_tr()
        return (lambda: None)

    def ln_pipeline(n, make, pre=None, hook_b=None):
        stg = [None] * n
        for it in range(n + 3):
            if it < n:
                if pre is not None:
                    pre(it)
                stg[it] = make(it)
                stg[it][0]()
            if 0 <= it - 1 < n:
                stg[it - 1][1]()
                if hook_b is not None:
                    hook_b(it - 1)
            if 0 <= it - 2 < n:
                stg[it - 2][2]()
            if 0 <= it - 3 < n:
                stg[it - 3][3]()

    def load_ln(gname, bname, l):
        g_ap = Din[gname] if l is None else Din[gname][l]
        b_ap = Din[bname] if l is None else Din[bname][l]
        dma("sp", lng, g_ap.partition_broadcast(128), (), ["lng"])
        dma("sp", lnb, b_ap.partition_broadcast(128), (), ["lnb"])

    def cexp_table(o_re, o_im, lr, li, shape, sign, tmp, kp):
        mag, th, kf, sn, ki = tmp
        r = lambda *n: [(kp, x) for x in n]
        act(mag, lr, AF.Exp, r("lr"), r("mag"), scale=float(sign))
        for which, o in ((0, o_im), (1, o_re)):
            ts(th, li, float(sign), (math.pi / 2 if which else 0.0), ALU.mult, ALU.add, r("li"), r("th"))
            ts(kf, th, 1.0 / TWO_PI, None, ALU.mult, None, r("th"), r("kf"))
            cp("dve", ki, kf, r("kf"), r("ki"))
            cp("dve", kf, ki, r("ki"), r("kf"))
            stt(th, kf, -TWO_PI, th, ALU.mult, ALU.add, r("kf", "th"), r("th"))
            ts(th, th, 3.14159, -3.14159, ALU.min, ALU.max, r("th"), r("th"))
            act(sn, th, AF.Sin, r("th"), r("sn"))
            tt(o, sn, mag, ALU.mult, r("sn", "mag"), r("o%d" % which))

    for SQ in range(2):
        CUR["T0"] = S * SQ
        A.mark()
        lng = A.f32(1024)
        lnb = A.f32(1024)
        ln_tmp = [(A.f32(2, 6), A.f32(2), A.f32(1), A.f32(1), A.f32(1), A.f32(1024), A.f32(1024)) for _ in range(4)]
        xt = [A.f32(1024) for _ in range(4)]
        load_ln("ln0_g", "ln0_b", None)
        t_lo, t_hi = 16 * SQ, 16 * SQ + 16
        for t in range(t_lo, t_lo + 4):
            dma("sp", xt[t % 4], Din["x"][128 * t:128 * t + 128, :], (), [("xt", t % 4)])

        def ln0_hook(i):
            t = t_lo + i + 4
            if t < t_hi:
                dma("sp", xt[t % 4], Din["x"][128 * t:128 * t + 128, :], (), [("xt", t % 4)])

        ln_pipeline(16, lambda i: ln_tile(xt[(t_lo + i) % 4], ("xt", (t_lo + i) % 4), lng, lnb, 128 * (t_lo + i), (t_lo + i) % 4, staged=True, geng="dve"),
                    hook_b=ln0_hook)
        A.release("ln0")
        P.barrier()

        for l in range(2):
            A.mark()
            A.mark()
            L = "L%d" % l
            Wu = A.bf16(8, 256)
            dma("pool", Wu, Din["w_in"][l].rearrange("(kc p) n -> p kc n", p=128)[:, :, 1536:1792], (), ["Wu"])
            LP0 = A.off
            P_re = A.f32(1024); P_im = A.f32(1024)
            Q_re = A.f32(8, 128); Q_im = A.f32(8, 128)
            Q1_re = A.bf16(8, 128); Q1_im = A.bf16(8, 128)
            AL_re = A.f32(8); AL_im = A.f32(8)
            AB_re = A.f32(8); AB_im = A.f32(8)
            Bbig = A.bf16(2, 2, 512)
            Cst_re = A.f32(8, 32); Cst_nim = A.f32(8, 32)
            Cst_re_b = A.bf16(8, 32); Cst_nim_b = A.bf16(8, 32)
            Ddiag = A.bf16(2, 128)
            wglu = A.bf16(2, 256)
            bglu_b = A.f32(256)
            WAbd = A.bf16(2, 128); WXbd = A.bf16(2, 128)
            lrup = A.f32(2, 12)
            mixg_b = A.f32(1024)
            LP1 = A.off
            P_real = P
            if SQ == 1:
                P = Prog(nc)
            A.mark()
            lam_tm = A.f32(2, 1024)
            dt_tm = A.f32(1024)
            tmpA = (A.f32(1024), A.f32(1024), A.f32(1024), A.f32(1024), A.i32(1024))
            prm = A.f32(8, 8)
            Bst = A.f32(2, 8, 16)
            Bbar = A.f32(2, 8, 16)
            Bwide = A.f32(2, 8, 128)
            Cwide = A.f32(2, 8, 128)
            fcoef = A.f32(8, 8)
            wtmp = A.f32(2, 256)
            wbd = A.f32(2, 2, 128)
            dcol = A.f32(2)
            are_d = Din["ssm_a_re"][l]; aim_d = Din["ssm_a_im"][l]; ldt_d = Din["ssm_log_dt"][l]
            flat = lambda ap_: ap_.rearrange("g p -> (g p)")
            dma("sp", lam_tm[:, 0, :], flat(are_d).partition_broadcast(128), (), [(L, "lamtm")])
            dma("sp", lam_tm[:, 1, :], flat(aim_d).partition_broadcast(128), (), [(L, "lamtm")])
            ldt_t = ldt_d.tensor
            ldt16 = A.f32(16)
            dma("sp", ldt16, ldt_d.partition_broadcast(128), (), [(L, "ldt16")])
            act(ldt16, ldt16, AF.Exp, [(L, "ldt16")], [(L, "ldt16")])
            cp("dve", resh(dt_tm, [16, 64]), ldt16.unsqueeze(2).to_broadcast([128, 16, 64]), [(L, "ldt16")], [(L, "dttm")])
            tt(lam_tm[:, 0, :], lam_tm[:, 0, :], dt_tm, ALU.mult, [(L, "lamtm"), (L, "dttm")], [(L, "lamtm")])
            tt(lam_tm[:, 1, :], lam_tm[:, 1, :], dt_tm, ALU.mult, [(L, "lamtm"), (L, "dttm")], [(L, "lamtm")])
            ts(lam_tm[:, 0, :], lam_tm[:, 0, :], iota_col, None, ALU.mult, None, [(L, "lamtm")], [(L, "lamtm")])
            ts(lam_tm[:, 1, :], lam_tm[:, 1, :], iota_col, None, ALU.mult, None, [(L, "lamtm")], [(L, "lamtm")])
            P.op("dve", lambda e: e.tensor_copy(out=tmpA[0][:, 0:1], in_=lam_tm[:, 0, 0:1]), [(L, "lamtm")], [("cxP", "lr"), ("cxP", "li")])
            cexp_table(P_re, P_im, lam_tm[:, 0, :], lam_tm[:, 1, :], None, -1.0, tmpA, "cxP")
            fm = lambda ap_: ap_.rearrange("g p -> (g p)").rearrange("(k q) -> q k", q=128)
            dma("sp", prm[:, :, 0], fm(are_d), (), [(L, "prm")], slow=True)
            dma("sp", prm[:, :, 1], fm(aim_d), (), [(L, "prm")], slow=True)
            for half in range(2):
                srcd = bass.AP(ldt_t, ldt_d.offset + half, [[0, 64], [2, 8]])
                dma("sp", prm[64 * half:64 * half + 64, :, 2], srcd, (), [(L, "prm")], slow=True)
            act(prm[:, :, 3], prm[:, :, 2], AF.Exp, [(L, "prm")], [(L, "prm")])
            tt(prm[:, :, 4], prm[:, :, 0], prm[:, :, 3], ALU.mult, [(L, "prm")], [(L, "prm")])
            tt(prm[:, :, 5], prm[:, :, 1], prm[:, :, 3], ALU.mult, [(L, "prm")], [(L, "prm")])
            lre = prm[:, :, 4]; lim = prm[:, :, 5]
            argr = resh(tmpA[0], [8, 128]); argi = resh(tmpA[1], [8, 128])
            qa_r = A.f32(8, 128); qa_i = A.f32(8, 128)
            tmpB = (A.f32(1024), A.f32(1024), A.f32(1024), A.f32(1024), A.i32(1024))
            irb = iota_row.unsqueeze(1).to_broadcast([128, 8, 128])
            tt(qa_r, irb, lre.unsqueeze(2).to_broadcast([128, 8, 128]), ALU.mult, [(L, "prm"), "iota_row"], [("cxQ", "lr")])
            tt(qa_i, irb, lim.unsqueeze(2).to_broadcast([128, 8, 128]), ALU.mult, [(L, "prm"), "iota_row"], [("cxQ", "li")])
            cexp_table(Q_re.rearrange("p a b -> p (a b)"), Q_im.rearrange("p a b -> p (a b)"),
                       qa_r.rearrange("p a b -> p (a b)"), qa_i.rearrange("p a b -> p (a b)"), None, 1.0, tmpB, "cxQ")
            sm = [A.f32(8) for _ in range(4)] + [A.i32(8)]
            cp("dve", sm[0][:, 0:1], lre[:, 0:1], [(L, "prm")], [("cxA", "lr"), ("cxA", "li")])
            cexp_table(AB_re, AB_im, lre, lim, None, 1.0, sm, "cxA")
            l128 = A.f32(2, 8)
            ts(l128[:, 0, :], lre, 128.0, None, ALU.mult, None, [(L, "prm")], [("cxL", "lr")])
            ts(l128[:, 1, :], lim, 128.0, None, ALU.mult, None, [(L, "prm")], [("cxL", "li")])
            sm2 = [A.f32(8) for _ in range(4)] + [A.i32(8)]
            cexp_table(AL_re, AL_im, l128[:, 0, :], l128[:, 1, :], None, 1.0, sm2, "cxL")
            t1 = resh(tmpB[0], [8, 128]); t2 = resh(tmpB[1], [8, 128])
            abr = AB_re.unsqueeze(2).to_broadcast([128, 8, 128]); abi = AB_im.unsqueeze(2).to_broadcast([128, 8, 128])
            qk = [("cxQ", "o0"), ("cxQ", "o1"), ("cxA", "o0"), ("cxA", "o1"), ("cxQ", "sn"), ("cxQ", "mag")]
            tt(t1, Q_re, abr, ALU.mult, qk, [(L, "t1")])
            tt(t2, Q_im, abi, ALU.mult, qk, [(L, "t2")])
            tt(Q1_re, t1, t2, ALU.subtract, [(L, "t1"), (L, "t2")], [(L, "Q1")])
            tt(t1, Q_re, abi, ALU.mult, qk + [(L, "Q1")], [(L, "t1")])
            tt(t2, Q_im, abr, ALU.mult, qk + [(L, "Q1")], [(L, "t2")])
            tt(Q1_im, t1, t2, ALU.add, [(L, "t1"), (L, "t2")], [(L, "Q1")])
            are = prm[:, :, 0]; aim = prm[:, :, 1]
            fk = [(L, "prm"), ("cxA", "o0"), ("cxA", "o1"), (L, "fc")]
            f = lambda i: fcoef[:, :, i]
            ts(f(2), AB_re, -1.0, None, ALU.add, None, fk, [(L, "fc")])
            tt(f(3), are, are, ALU.mult, fk, [(L, "fc")])
            tt(f(4), aim, aim, ALU.mult, fk, [(L, "fc")])
            tt(f(3), f(3), f(4), ALU.add, fk, [(L, "fc")])
            recip(f(3), f(3), fk, [(L, "fc")])
            tt(f(4), f(2), are, ALU.mult, fk, [(L, "fc")])
            tt(f(5), AB_im, aim, ALU.mult, fk, [(L, "fc")])
            tt(f(4), f(4), f(5), ALU.add, fk, [(L, "fc")])
            tt(f(0), f(4), f(3), ALU.mult, fk, [(L, "fc")])
            tt(f(4), AB_im, are, ALU.mult, fk, [(L, "fc")])
            tt(f(5), f(2), aim, ALU.mult, fk, [(L, "fc")])
            tt(f(4), f(4), f(5), ALU.subtract, fk, [(L, "fc")])
            tt(f(1), f(4), f(3), ALU.mult, fk, [(L, "fc")])
            bsrc = lambda ap_: ap_.rearrange("(k gl) p h -> (gl p) k h", gl=2)
            dma("sp", Bst[:, 0], bsrc(Din["ssm_b_re"][l]), (), [(L, "Bst")])
            dma("sp", Bst[:, 1], bsrc(Din["ssm_b_im"][l]), (), [(L, "Bst")])
            frb = f(0).unsqueeze(2).to_broadcast([128, 8, 16]); fib = f(1).unsqueeze(2).to_broadcast([128, 8, 16])
            u1 = resh(tmpB[2][:, 0:128], [8, 16]); u2 = resh(tmpB[3][:, 0:128], [8, 16])
            bk = [(L, "Bst"), (L, "fc"), (L, "u1"), (L, "u2"), (L, "Bbar")]
            tt(u1, Bst[:, 0], frb, ALU.mult, bk, [(L, "u1")])
            tt(u2, Bst[:, 1], fib, ALU.mult, bk, [(L, "u2")])
            tt(Bbar[:, 0], u1, u2, ALU.subtract, bk, [(L, "Bbar")])
            tt(u1, Bst[:, 1], frb, ALU.mult, bk, [(L, "u1")])
            tt(u2, Bst[:, 0], fib, ALU.mult, bk, [(L, "u2")])
            tt(Bbar[:, 1], u1, u2, ALU.add, bk, [(L, "Bbar")])
            memset("pool", Bwide.rearrange("p a b c -> p (a b c)"), 0.0, [(L, "Bwide")])
            bw_t = Bwide.tensor
            for ri in range(2):
                for gl in range(2):
                    base = Bwide.offset + (64 * gl) * SBW + ri * 1024 + 16 * gl
                    o = bass.AP(bw_t, base, [[SBW, 64], [512, 2], [160, 4], [1, 16]])
                    i_ = resh(Bbar[64 * gl:64 * gl + 64, ri].rearrange("p k h -> p (k h)"), [2, 4, 16])
                    cp("dve", o, i_, [(L, "Bbar"), (L, "Bwide")], [(L, "Bwide")])
            for ri in range(2):
                for k in range(8):
                    j, kl = k // 4, k % 4
                    b = 4 + (k % 2)
                    tr(PSB[b][:, 0:128], Bwide[:, ri, k, :], identf, [(L, "Bwide"), "identf"], [PK(b)])
                    cp("act", Bbig[:, ri, j, 128 * kl:128 * kl + 128], PSB[b][:, 0:128], [PK(b)], [(L, "Bbig")])
            memset("pool", Cwide.rearrange("p a b c -> p (a b c)"), 0.0, [(L, "Cwide")])
            for ri, nm in ((0, "ssm_c_re"), (1, "ssm_c_im")):
                cd = Din[nm][l]
                for gl in range(2):
                    src_ = cd.rearrange("(k gl) h p -> gl h k p", gl=2)[gl]
                    dma("sp", Cwide[16 * gl:16 * gl + 16, ri, :, 64 * gl:64 * gl + 64], src_, [(L, "Cwide")], [(L, "Cwide")])
            for ri in range(2):
                for k in range(8):
                    b = 4 + (k % 2)
                    tr(PSB[b][:, 0:32], Cwide[0:32, ri, k, :], identf[0:32, 0:32], [(L, "Cwide"), "identf"], [PK(b)])
                    if ri == 0:
                        cp("act", Cst_re[:, k, :], PSB[b][:, 0:32], [PK(b)], [(L, "Cst")])
                    else:
                        act(Cst_nim[:, k, :], PSB[b][:, 0:32], AF.Copy, [PK(b)], [(L, "Cst")], scale=-1.0)
            cp("dve", Cst_re_b, Cst_re, [(L, "Cst")], [(L, "Cstb")])
            cp("dve", Cst_nim_b, Cst_nim, [(L, "Cst")], [(L, "Cstb")])
            dma("sp", dcol, Din["ssm_d"][l].rearrange("(j p) -> p j", p=128), (), [(L, "dcol")], slow=True)
            for j in range(2):
                ts(Ddiag[:, j, :], identf, dcol[:, j:j + 1], None, ALU.mult, None, [(L, "dcol"), "identf"], [(L, "Ddiag")])
            dma("pool", wglu, Din["ssm_w_glu"][l].rearrange("(j p) n -> p j n", p=128), (), [(L, "wglu")])
            dma("sp", bglu_b, Din["ssm_b_glu"][l].partition_broadcast(128), (), [(L, "bglu")])
            pcol = lambda nm: Din[nm][l].rearrange("(j p) -> p j", p=128)
            for jj in range(4):
                dma("sp", lrup[:, :, jj], Din["lru_conv_w"][l][jj].rearrange("(j p) -> p j", p=128), (), [(L, "lrup")], slow=True)
            for ci, nm in ((4, "lru_conv_b"), (5, "lru_b_a"), (6, "lru_b_x"), (7, "lru_lam")):
                dma("sp", lrup[:, :, ci], pcol(nm), (), [(L, "lrup")], slow=True)
            dma("sp", lrup[:, :, 9], Din["mix_g"][l][768:1024].rearrange("(j p) -> p j", p=128), (), [(L, "lrup")], slow=True)
            act(lrup[:, :, 10], lrup[:, :, 7], AF.Exp, [(L, "lrup")], [(L, "lrup")], scale=-1.0)
            act(lrup[:, :, 10], lrup[:, :, 10], AF.Ln, [(L, "lrup")], [(L, "lrup")], bias=C1, scale=1.0)
            ts(lrup[:, :, 8], lrup[:, :, 10], -8.0, None, ALU.mult, None, [(L, "lrup")], [(L, "lrup")])
            ts(lrup[:, :, 11], lrup[:, :, 8], 2.0, None, ALU.mult, None, [(L, "lrup")], [(L, "lrup")])
            memset("pool", wbd.rearrange("p a b c -> p (a b c)"), 0.0, [(L, "wbd")])
            for wi, nm in ((0, "lru_w_a"), (1, "lru_w_x")):
                for hh in range(4):
                    fc, hl = hh // 2, hh % 2
                    dma("sp", wbd[64 * hl:64 * hl + 64, wi, fc, 64 * hl:64 * hl + 64], Din[nm][l][hh], [(L, "wbd")], [(L, "wbd")])
            cp("dve", WAbd, wbd[:, 0], [(L, "wbd")], [(L, "WAbd")])
            cp("dve", WXbd, wbd[:, 1], [(L, "wbd")], [(L, "WAbd")])
            dma("sp", mixg_b, Din["mix_g"][l].partition_broadcast(128), (), [(L, "mixgb")])
            P.barrier()
            A.release("prep")
            P = P_real
            assert LP1 - LP0 <= PREPW, (LP1 - LP0)
            if SQ == 0:
                dma("sp", prepbuf[l][:, 0:LP1 - LP0], SBT[:, LP0:LP1], (), [("prepbuf", l)])
            else:
                dma("sp", SBT[:, LP0:LP1], prepbuf[l][:, 0:LP1 - LP0], [("prepbuf", l)], [("prepld", l)])
                P.barrier()

            for s in (SQ,):
                T0 = S * s
                A.mark()
                Yattn = A.bf16(16, 512)
                Yssm = A.bf16(16, 256)
                Ylru = A.bf16(16, 256)
                ssqS = A.f32(16)
                A.mark()
                Wqkv0 = [A.bf16(8, 256) for _ in range(3)]
                Wxl2 = [A.bf16(8, 128) for _ in range(2)]; Wgl2 = [A.bf16(8, 128) for _ in range(2)]
                A.mark()
                uT = A.bf16(2, S)
                paB = [[A.bf16(1024) for _ in range(4)] for _ in range(2)]
                vB = [[A.bf16(8, 128) for _ in range(4)] for _ in range(2)]
                car = A.f32(2, 8)
                cart = [A.f32(8) for _ in range(4)]
                E12 = A.f32(2, 8, 32)
                E12b = A.bf16(2, 8, 32)
                ygl = [A.f32(256) for _ in range(2)]
                yglT = [A.bf16(2, 128) for _ in range(2)]
                sg = [A.f32(256) for _ in range(2)]
                w3 = Din["w_in"][l].rearrange("(kc p) n -> p kc n", p=128)
                for fc in range(2):
                    dma("pool", Wxl2[fc], w3[:, :, 1792 + 128 * fc:1792 + 128 * fc + 128], (), [("Wxl", fc)])
                    dma("pool", Wgl2[fc], w3[:, :, 2048 + 128 * fc:2048 + 128 * fc + 128], (), [("Wgl", fc)])
                for wi_ in range(3):
                    dma("pool", Wqkv0[wi_], w3[:, :, 512 * wi_:512 * wi_ + 256], (), [("Wqkv", 0)])
                for fc in range(2):
                    for tg in range(4):
                        b = (fc * 4 + tg) % 2
                        for kc in range(8):
                            mm(PSB[b][:, :], Wu[:, kc, 128 * fc:128 * fc + 128], hT[:, kc, 512 * tg:512 * tg + 512], kc == 0, kc == 7,
                               ["Wu", ("hT", 0)], [PK(b)])
                        cp("act" if tg % 2 == 0 else "dve", uT[:, fc, 512 * tg:512 * tg + 512], PSB[b][:, :], [PK(b)], ["uT"])
                memset("dve", car.rearrange("p a b -> p (a b)"), 0.0, ["car"])

                def s5_A(c):
                    sl = c % 2
                    tk = slice(128 * c, 128 * c + 128)
                    for ri in range(2):
                        for j in range(2):
                            bnk = 2 * ri + j
                            mm(PSB[bnk][:, :], uT[:, j, tk], Bbig[:, ri, j, :], True, True, ["uT", (L, "Bbig")], [PK(bnk)])
                    for j in range(2):
                        cs = slice(512 * j, 512 * j + 512)
                        bur, bui = PSB[j][:, :], PSB[2 + j][:, :]
                        tt(paB[sl][0][:, cs], bur, P_re[:, cs], ALU.mult, [PK(j)], [("pa", sl, 0, j)])
                        stt(paB[sl][1][:, cs], bui, -1.0, P_im[:, cs], ALU.mult, ALU.mult, [PK(2 + j)], [("pa", sl, 1, j)])
                        tt(paB[sl][2][:, cs], bur, P_im[:, cs], ALU.mult, [PK(j)], [("pa", sl, 2, j)])
                        tt(paB[sl][3][:, cs], bui, P_re[:, cs], ALU.mult, [PK(2 + j)], [("pa", sl, 3, j)])

                def s5_B(c):
                    sl = c % 2
                    for ri in range(2):
                        ia, ib = (0, 1) if ri == 0 else (2, 3)
                        for k in range(8):
                            bnk = 4 + 2 * ri + k // 4
                            o_ = PSB[bnk][:, 128 * (k % 4):128 * (k % 4) + 128]
                            mm(o_, paB[sl][ia][:, 128 * k:128 * k + 128], tri_cs, True, False, [("pa", sl, ia, k // 4), "tri_cs"], [PK(bnk)])
                            mm(o_, paB[sl][ib][:, 128 * k:128 * k + 128], tri_cs, False, True, [("pa", sl, ib, k // 4), "tri_cs"], [PK(bnk)])
                    for hf in range(2):
                        zr = resh(PSB[4 + hf][:, :], [4, 128]); zi = resh(PSB[6 + hf][:, :], [4, 128])
                        ks = slice(4 * hf, 4 * hf + 4)
                        tt(vB[sl][0][:, ks, :], zr, Q_re[:, ks, :], ALU.mult, [PK(4 + hf)], [("vB", sl, 0, hf)])
                        stt(vB[sl][1][:, ks, :], zi, -1.0, Q_im[:, ks, :], ALU.mult, ALU.mult, [PK(6 + hf)], [("vB", sl, 1, hf)])
                        tt(vB[sl][2][:, ks, :], zr, Q_im[:, ks, :], ALU.mult, [PK(4 + hf)], [("vB", sl, 2, hf)])
                        tt(vB[sl][3][:, ks, :], zi, Q_re[:, ks, :], ALU.mult, [PK(6 + hf)], [("vB", sl, 3, hf)])

                def s5_Cpre(c):
                    xr = car[:, 0, :].unsqueeze(2).to_broadcast([128, 8, 32]); xi = car[:, 1, :].unsqueeze(2).to_broadcast([128, 8, 32])
                    ek = ["car", "E12", (L, "Cst")]
                    tt(E12[:, 0], Cst_re, xr, ALU.mult, ek, ["E12"], eng="pool")
                    tt(E12[:, 1], Cst_nim, xi, ALU.mult, ek, ["E12"], eng="pool")
                    tt(E12b[:, 0], E12[:, 0], E12[:, 1], ALU.add, ["E12"], ["E12b"], eng="pool")
                    tt(E12[:, 0], Cst_re, xi, ALU.mult, ek, ["E12"], eng="pool")
                    tt(E12[:, 1], Cst_nim, xr, ALU.mult, ek, ["E12"], eng="pool")
                    tt(E12b[:, 1], E12[:, 1], E12[:, 0], ALU.subtract, ["E12"], ["E12b"], eng="pool")

                def s5_C(c):
                    sl = c % 2
                    tk = slice(128 * c, 128 * c + 128)
                    XTk = [("vB", sl, i_, hf_) for i_ in range(4) for hf_ in range(2)]
                    yb = 0
                    for j in range(2):
                        mm(PSB[yb][:, 128 * j:128 * j + 128], uT[:, j, tk], Ddiag[:, j, :], True, False, ["uT", (L, "Ddiag")], [PK(yb)])
                        for kk in range(4):
                            k = 4 * j + kk
                            oc = PSB[yb][:, 32 * k:32 * k + 32]
                            mm(oc, vB[sl][0][:, k, :], Cst_re_b[:, k, :], False, False, XTk + [(L, "Cstb")], [PK(yb)])
                            mm(oc, vB[sl][1][:, k, :], Cst_re_b[:, k, :], False, False, XTk + [(L, "Cstb")], [PK(yb)])
                            mm(oc, vB[sl][2][:, k, :], Cst_nim_b[:, k, :], False, False, XTk + [(L, "Cstb")], [PK(yb)])
                            mm(oc, vB[sl][3][:, k, :], Cst_nim_b[:, k, :], False, False, XTk + [(L, "Cstb")], [PK(yb)])
                            mm(oc, Q1_re[:, k, :], E12b[:, 0, k, :], False, False, [(L, "Q1"), "E12b"], [PK(yb)])
                            mm(oc, Q1_im[:, k, :], E12b[:, 1, k, :], False, kk == 3, [(L, "Q1"), "E12b"], [PK(yb)])
                    vl = [vB[sl][i_][:, :, 127] for i_ in range(4)]
                    ck = ["car", "cart", ("cxL", "o0"), ("cxL", "o1")] + XTk
                    tt(cart[0], AL_re, car[:, 0, :], ALU.mult, ck, ["cart"], eng="pool")
                    tt(cart[1], AL_im, car[:, 1, :], ALU.mult, ck, ["cart"], eng="pool")
                    tt(cart[2], AL_re, car[:, 1, :], ALU.mult, ck, ["cart"], eng="pool")
                    tt(cart[3], AL_im, car[:, 0, :], ALU.mult, ck, ["cart"], eng="pool")
                    tt(cart[0], cart[0], cart[1], ALU.subtract, ["cart"], ["cart"], eng="pool")
                    tt(cart[2], cart[2], cart[3], ALU.add, ["cart"], ["cart"], eng="pool")
                    tt(cart[0], cart[0], vl[0], ALU.add, ["cart"] + XTk, ["cart"], eng="pool")
                    tt(cart[2], cart[2], vl[2], ALU.add, ["cart"] + XTk, ["cart"], eng="pool")
                    tt(car[:, 0, :], cart[0], vl[1], ALU.add, ["cart", "E12"] + XTk, ["car"], eng="pool")
                    tt(car[:, 1, :], cart[2], vl[3], ALU.add, ["cart", "E12"] + XTk, ["car"], eng="pool")
                    act(ygl[sl], PSB[yb][:, 0:256], AF.Gelu_apprx_tanh, [PK(yb)], [("ygl", sl)])
                    tb = 1
                    for j in range(2):
                        tr(PSB[tb][:, 128 * j:128 * j + 128], ygl[sl][:, 128 * j:128 * j + 128], identf, [("ygl", sl), "identf"], [PK(tb)])
                    cp("act", yglT[sl], resh(PSB[tb][:, 0:256], [2, 128]), [PK(tb)], [("yglT", sl)])
                    gb2 = 2
                    for j in range(2):
                        mm(PSB[gb2][:, 0:256], yglT[sl][:, j, :], wglu[:, j, :], j == 0, j == 1, [("yglT", sl), (L, "wglu")], [PK(gb2)])
                    tt(sg[sl], PSB[gb2][:, 0:256], bglu_b, ALU.add, [PK(gb2), (L, "bglu")], [("sg", sl)])
                    act(sg[sl], sg[sl], AF.Sigmoid, [("sg", sl)], [("sg", sl)])

                def s5_Cpost(c):
                    sl = c % 2
                    tt(ygl[sl], ygl[sl], sg[sl], ALU.mult, [("ygl", sl), ("sg", sl), ("yglT", sl)], [("ygl", sl)], eng="pool")
                    cp("pool", Yssm[:, c, :], ygl[sl], [("ygl", sl)], [("Yssm", c)])
                    act(sg[sl], ygl[sl], AF.Square, [("ygl", sl), ("sg", sl)], [("sg", sl)], accum=ssqS[:, c:c + 1])
                    if DEBUG and l == 0:
                        dma("sp", dbg["ys"][T0 + 128 * c:T0 + 128 * c + 128, :], ygl[sl], [("ygl", sl)], [("dbgys", c)])

                s5_A(0)
                for c in range(18):
                    if 0 <= c - 1 < 16:
                        s5_Cpre(c - 1)
                    if 0 <= c - 2 < 16:
                        s5_Cpost(c - 2)
                    if c + 1 < 16:
                        s5_A(c + 1)
                    if c < 16:
                        s5_B(c)
                    if 0 <= c - 1 < 16:
                        s5_C(c - 1)
                A.release("s5")
                P.barrier()

                A.mark()
                xpad2 = [A.f32(S + 4) for _ in range(2)]
                glg2 = [A.bf16(S) for _ in range(2)]
                xcb2 = [A.bf16(S) for _ in range(2)]
                cacc = A.f32(512)
                rr = [A.f32(512) for _ in range(2)]
                ii = [A.f32(512) for _ in range(2)]
                aa = [A.f32(512) for _ in range(2)]
                bb = [A.f32(512) for _ in range(2)]
                a2 = [A.f32(512) for _ in range(2)]
                hs2 = [[A.f32(512) for _ in range(2)] for _ in range(2)]
                hg_ = [A.f32(512) for _ in range(2)]
                w3 = Din["w_in"][l].rearrange("(kc p) n -> p kc n", p=128)
                for fc in range(2):
                    memset("dve", xpad2[fc][:, 0:4], 0.0, [("xpad", fc)])
                for fc in range(2):
                    xpad = xpad2[fc]; glg = glg2[fc]; xcb = xcb2[fc]
                    pc = lambda i, fc=fc: lrup[:, fc, i:i + 1]
                    for tg in range(4):
                        for which, W_ in ((0, Wxl2[fc]), (1, Wgl2[fc])):
                            b = which
                            for kc in range(8):
                                mm(PSB[b][:, :], W_[:, kc, :], hT[:, kc, 512 * tg:512 * tg + 512], kc == 0, kc == 7,
                                   [("Wxl", fc), ("Wgl", fc)], [PK(b)])
                            if which == 0:
                                cp("dve", xpad[:, 4 + 512 * tg:4 + 512 * tg + 512], PSB[b][:, :], [PK(b), ("xpad", fc)], [("xpad", fc)])
                            else:
                                act(glg[:, 512 * tg:512 * tg + 512], PSB[b][:, :], AF.Gelu_apprx_tanh, [PK(b)], [("glg", fc)])
                    for tg in range(4):
                        tk = slice(512 * tg, 512 * tg + 512)
                        ts(cacc, xpad[:, 4 + 512 * tg:4 + 512 * tg + 512], pc(3), pc(4), ALU.mult, ALU.add, [("xpad", fc), (L, "lrup"), "cacc"], ["cacc"])
                        for jj in range(3):
                            o_ = cacc if jj < 2 else xcb[:, tk]
                            stt(o_, xpad[:, 1 + jj + 512 * tg:1 + jj + 512 * tg + 512], pc(jj), cacc, ALU.mult, ALU.add, [("xpad", fc), (L, "lrup"), "cacc"],
                                ["cacc"] if jj < 2 else [("xcb", fc, tg)])
                def lru_step(tg, fc):
                    xcb = xcb2[fc]; glg = glg2[fc]
                    pc = lambda i, fc=fc: lrup[:, fc, i:i + 1]
                    sl = fc
                    tk = slice(512 * tg, 512 * tg + 512)
                    hs = hs2[fc]
                    hsl = tg % 2
                    ops = []
                    ops.append(lambda: mm(PSB[2 + sl][:, :], WAbd[:, fc, :], xcb[:, tk], True, True, [(L, "WAbd"), ("xcb", fc, tg)], [PK(2 + sl)]))
                    ops.append(lambda: mm(PSB[4 + sl][:, :], WXbd[:, fc, :], xcb[:, tk], True, True, [(L, "WAbd"), ("xcb", fc, tg)], [PK(4 + sl)]))
                    ops.append(lambda: act(rr[sl], PSB[2 + sl][:, :], AF.Sigmoid, [PK(2 + sl)], [("rr", sl)], bias=pc(5), scale=1.0))
                    ops.append(lambda: act(ii[sl], PSB[4 + sl][:, :], AF.Sigmoid, [PK(4 + sl)], [("ii", sl)], bias=pc(6), scale=1.0))
                    ops.append(lambda: act(aa[sl], rr[sl], AF.Exp, [("rr", sl)], [("aa", sl)], scale=pc(8)))
                    ops.append(lambda: act(a2[sl], rr[sl], AF.Exp, [("rr", sl)], [("a2", sl)], scale=pc(11)))
                    ops.append(lambda: ts(a2[sl], a2[sl], -1.0, 1.0, ALU.mult, ALU.add, [("a2", sl)], [("a2", sl)]))
                    ops.append(lambda: tt(ii[sl], ii[sl], xcb[:, tk], ALU.mult, [("ii", sl), ("xcb", fc, tg)], [("ii", sl)]))
                    ops.append(lambda: ts(a2[sl], a2[sl], 0.0, None, ALU.max, None, [("a2", sl)], [("a2", sl)]))
                    ops.append(lambda: act(a2[sl], a2[sl], AF.Sqrt, [("a2", sl)], [("a2", sl)]))
                    ops.append(lambda: tt(bb[sl], ii[sl], a2[sl], ALU.mult, [("ii", sl), ("a2", sl)], [("bb", sl)]))
                    init = 0.0 if tg == 0 else hs[1 - hsl][:, 511:512]
                    ops.append(lambda: P.op("dve", (lambda e, o=hs[hsl], d0=aa[sl], d1=bb[sl], init=init: e.tensor_tensor_scan(
                        out=o, data0=d0, data1=d1, initial=init, op0=ALU.mult, op1=ALU.add)),
                        [("aa", sl), ("bb", sl), ("hs", fc, 1 - hsl)], [("hs", fc, hsl)]))
                    ops.append(lambda: tt(hg_[sl], hs[hsl], glg[:, tk], ALU.mult, [("hs", fc, hsl), ("glg", fc)], [("hg", sl)]))
                    tb_ = 6 + sl

                    def trs():
                        for i4 in range(4):
                            tr(PSB[tb_][:, 128 * i4:128 * i4 + 128], hg_[sl][:, 128 * i4:128 * i4 + 128], identf, [("hg", sl), "identf"], [PK(tb_)])
                    ops.append(trs)
                    ops.append(lambda: cp("act", Ylru[:, 4 * tg:4 * tg + 4, 128 * fc:128 * fc + 128], resh(PSB[tb_][:, :], [4, 128]), [PK(tb_)], [("Ylru", tg)]))
                    return ops

                for tg in range(4):
                    o0 = lru_step(tg, 0); o1 = lru_step(tg, 1)
                    for a_, b_ in zip(o0, o1):
                        a_(); b_()
                A.release("lru")
                P.barrier()

                A.mark()
                Wqkv = [Wqkv0, [A.bf16(8, 256) for _ in range(3)]]
                w3 = Din["w_in"][l].rearrange("(kc p) n -> p kc n", p=128)

                def load_attn_w(hp_, rd=()):
                    for wi_ in range(3):
                        dma("pool", Wqkv[hp_][wi_], w3[:, :, 512 * wi_ + 256 * hp_:512 * wi_ + 256 * hp_ + 256], rd, [("Wqkv", hp_)])

                for hp in range(2):
                    A.mark()
                    qa = A.bf16(4, S)
                    ka = A.bf16(4, S)
                    Va = A.bf16(16, 4, 65)
                    Wq, Wk, Wv = Wqkv[hp]
                    pT = [A.bf16(512) for _ in range(4)]
                    gB = A.f32(16, 4, 8)
                    negc = A.f32(16, 8); gA = A.f32(16, 8)
                    kbias = A.f32(4, 16)
                    MBA = A.bf16(16, 4, 72)
                    gmA = A.f32(16, 4, 8); top8A = A.f32(16, 4, 8); t1A = A.f32(16, 4, 8)
                    km32 = A.f32(4, 8)
                    kmb = A.bf16(4, 8)
                    rec = [A.f32(1) for _ in range(4)]
                    dma("sp", gB, Din["gB"][hp], (), ["gB"])
                    dma("sp", negc, Din["negc"], (), ["negc"])
                    dma("sp", gA, Din["gA"], (), ["gA"])
                    dma("sp", kbias, Din["kbias"][:, 4 * hp:4 * hp + 4, :], (), ["kbias"])
                    for hh in range(4):
                        dma("sp", ka[64:72, hh, :], Din["blockind"], (), [("ka", hh)])
                    memset("dve", MBA.rearrange("p a b c -> p (a b c)"), 0.0, ["MBA"])
                    memset("dve", Va.rearrange("p a b c -> p (a b c)"), 1.0, ["Va"])
                    for tg in range(4):
                        tok = slice(512 * tg, 512 * tg + 512)
                        tkl = slice(512 * tg, 512 * tg + 512)
                        for hh in range(4):
                            for which, W, dst in ((0, Wq, qa), (1, Wk, ka)):
                                b = (2 * hh + which) % 2
                                for kc in range(8):
                                    mm(PSB[b][0:64, :], W[:, kc, 64 * hh:64 * hh + 64], hT[:, kc, tok], kc == 0, kc == 7,
                                       [("Wqkv", hp), ("hT", 0)], [PK(b)])
                                if which == 0:
                                    act(dst[0:64, hh, tkl], PSB[b][0:64, :], AF.Copy, [PK(b)], [("qa", hh)], scale=0.125)
                                else:
                                    cp("dve", dst[0:64, hh, tkl], PSB[b][0:64, :], [PK(b)], [("ka", hh)])

                    def emit_vproj():
                        for tti in range(16):
                            b = 2 + (tti % 2)
                            for kc in range(8):
                                mm(PSB[b][:, 0:256], hT[:, kc, 128 * tti:128 * tti + 128], Wv[:, kc, :], kc == 0, kc == 7,
                                   [("Wqkv", hp), ("hT", 0)], [PK(b)])
                            cp("act", Va[:, tti, :, 0:64], resh(PSB[b][:, 0:256], [4, 64]), [PK(b), "Va"], ["Va"])
                        if hp == 0:
                            load_attn_w(1, ["Va"])
                    for hh in range(4):
                        P.op("dve", (lambda e, hh=hh: e.tensor_reduce(out=km32[0:64, hh, :], in_=resh(ka[0:64, hh, :], [8, 256]),
                                                                         axis=AX.X, op=ALU.add)), [("ka", hh)], ["km32"])
                    ts(kmb[0:64].rearrange("p a b -> p (a b)"), km32[0:64].rearrange("p a b -> p (a b)"), 1.0 / 256.0, None, ALU.mult, None,
                       ["km32"], ["kmb"])
                    gb_ = 6
                    for tti in range(16):
                        for hh in range(4):
                            mm(PSB[gb_][:, 32 * tti + 8 * hh:32 * tti + 8 * hh + 8], qa[0:64, hh, 128 * tti:128 * tti + 128], kmb[0:64, hh, :], True, True,
                               [("qa", hh), "kmb"], [PK(gb_)])
                    emit_vproj()
                    tt(gmA, resh(PSB[gb_][:, :], [16, 4, 8]), negc.unsqueeze(2).to_broadcast([128, 16, 4, 8]), ALU.add, [PK(gb_), "negc"], ["gmA"])
                    for tti in range(16):
                        for hh in range(4):
                            P.op("dve", (lambda e, o=top8A[:, tti, hh, :], i_=gmA[:, tti, hh, :]: e.max(out=o, in_=i_)), ["gmA"], [("top8A", tti)])
                    tt(t1A, gmA, top8A[:, :, :, 2:3].to_broadcast([128, 16, 4, 8]), ALU.is_ge, ["gmA"] + [("top8A", t_) for t_ in range(16)], ["t1A"])
                    tt(t1A, t1A, gA.unsqueeze(2).to_broadcast([128, 16, 4, 8]), ALU.mult, ["t1A", "gA"], ["t1A"])
                    tt(MBA[:, :, :, 64:72], t1A, gB, ALU.add, ["t1A", "gB", "MBA"], ["MBA"])
                    for tti in range(16):
                        tb = 7 if tti % 2 == 0 else 5
                        tpv = resh(psb(tb, 512), [4, 128])
                        for hh in range(4):
                            tr(tpv[0:72, hh, :], MBA[:, tti, hh, :], identb, ["MBA", "identb"], [PK(tb)])
                        cp("act" if tti % 2 == 0 else "dve", qa[64:72, :, 128 * tti:128 * tti + 128], tpv[64:72, :, :], [PK(tb)], [("qa", 0), ("qa", 1), ("qa", 2), ("qa", 3)])
                    units = [(hh, g, kt) for hh in range(4) for g in range(4) for kt in range(4 * g + 4)]
                    SBK = (0, 1, 6, 7)

                    def emit_S(i):
                        hh, g, kt = units[i]
                        j = kt - 4 * g
                        qc0 = 128 * j if j > 0 else 0
                        ncol = 512 - qc0
                        sb_ = SBK[i % 4]
                        diag = j >= 0
                        mm(PSB[sb_][:, 0:ncol], ka[0:72, hh, 128 * kt:128 * kt + 128],
                           qa[0:72, hh, 512 * g + qc0:512 * g + 512], True, not diag, [("ka", hh), ("qa", hh)], [PK(sb_)])
                        if diag:
                            mm(PSB[sb_][:, 0:128], identb, trimask, False, True, ["identb", "trimask"], [PK(sb_)])

                    def emit_rest(i):
                        hh, g, kt = units[i]
                        hg = 4 * hp + hh
                        j = kt - 4 * g
                        qc0 = 128 * j if j > 0 else 0
                        ncol = 512 - qc0
                        sb_ = SBK[i % 4]
                        sl = i % 4
                        act(pT[sl][:, 0:ncol], PSB[sb_][:, 0:ncol], AF.Exp, [PK(sb_), "kbias"], [("pT", sl)],
                            bias=kbias[:, hh, j + 12:j + 13], scale=1.0)
                        for c in range(ncol // 128):
                            jq = (qc0 // 128) + c
                            ob = 2 + jq
                            mm(PSB[ob][:, 0:65], pT[sl][:, 128 * c:128 * c + 128], Va[:, kt, hh, :], kt == 0, kt == 4 * g + jq,
                               [("pT", sl), "Va"], [PK(ob)])
                            if kt == 4 * g + jq:
                                tti = 4 * g + jq
                                recip(rec[jq], PSB[ob][:, 64:65], [PK(ob)], [("rec", jq)])
                                P.op("dve", (lambda e, o=Yattn[:, tti, 64 * hg:64 * hg + 64], i_=PSB[ob][:, 0:64], s_=rec[jq]:
                                             e.tensor_scalar(out=o, in0=i_, scalar1=s_, scalar2=None, op0=ALU.mult)),
                                     [PK(ob), ("rec", jq)], [("Yattn", tti)])

                    LOOK = 2
                    for i in range(min(LOOK, len(units))):
                        emit_S(i)
                    for i in range(len(units)):
                        if i + LOOK < len(units):
                            emit_S(i + LOOK)
                        emit_rest(i)
                    A.release("attn")
                    if hp == 1:
                        P.barrier()
                A.release()
                if DEBUG and l == 0:
                    A.mark()
                    dt_ = A.f32(512)
                    for tti in range(16):
                        cp("dve", dt_, Yattn[:, tti, :], [("Yattn", tti)], ["dt_"])
                        dma("sp", dbg["ya"][T0 + 128 * tti:T0 + 128 * tti + 128, :], dt_, ["dt_"], [("dbgya", tti)])
                    A.release()
                    P.barrier()

                A.release()

                A.mark()
                Wo = A.bf16(8, 1024)
                lng = A.f32(1024); lnb = A.f32(1024)
                ln_tmp = [(A.f32(2, 6), A.f32(2), A.f32(1), A.f32(1), A.f32(1), A.f32(1024), A.f32(1024)) for _ in range(3)]
                ht = [A.f32(1024) for _ in range(2)]
                acc = [A.f32(1024) for _ in range(2)]
                yn = [A.bf16(1024) for _ in range(2)]
                ynT = [A.bf16(8, 128) for _ in range(2)]
                rs = [A.f32(8) for _ in range(2)]
                sqt = [A.f32(512) for _ in range(2)]
                dma("pool", Wo, Din["w_out"][l].rearrange("(kc p) n -> p kc n", p=128), (), ["Wo"])
                load_ln("ln1_g", "ln1_b", l)

                def op_f1(c):
                    sl = c % 2
                    k = lambda n: ("op", n, sl)
                    act(sqt[0], Yattn[:, c, :], AF.Square, [("Yattn", c), "sqt0"], ["sqt0", k("rs0")], accum=rs[sl][:, 0:1])
                    act(sqt[1][:, 0:256], Ylru[:, c, :], AF.Square, [("Ylru", c // 4), "sqt1"], ["sqt1", k("rs2")], accum=rs[sl][:, 2:3])
                    cp("dve", rs[sl][:, 1:2], ssqS[:, c:c + 1], [], [k("rs1")])
                    ts(rs[sl][:, 0:1], rs[sl][:, 0:1], 1.0 / 512, None, ALU.mult, None, ["sqt0", k("rs0")], [k("rs0")])
                    ts(rs[sl][:, 1:3], rs[sl][:, 1:3], 1.0 / 256, None, ALU.mult, None, ["sqt1", k("rs1"), k("rs2")], [k("rsm")])
                    act(rs[sl][:, 3:6], rs[sl][:, 0:3], AF.Sqrt, [k("rs0"), k("rsm")], [k("rsq")], bias=CRMS, scale=1.0)
                    recip(rs[sl][:, 3:6], rs[sl][:, 3:6], [k("rsq")], [k("rstd")])
                    stt(yn[sl][:, 0:512], Yattn[:, c, :], rs[sl][:, 3:4], mixg_b[:, 0:512], ALU.mult, ALU.mult,
                        [("Yattn", c), k("rstd"), (L, "mixgb")], [k("yn")])
                    stt(yn[sl][:, 512:768], Yssm[:, c, :], rs[sl][:, 4:5], mixg_b[:, 512:768], ALU.mult, ALU.mult,
                        [("Yssm", c), k("rstd"), (L, "mixgb")], [k("yn")])
                    stt(yn[sl][:, 768:1024], Ylru[:, c, :], rs[sl][:, 5:6], mixg_b[:, 768:1024], ALU.mult, ALU.mult,
                        [("Ylru", c // 4), k("rstd"), (L, "mixgb")], [k("yn")])

                def op_f2(c):
                    sl = c % 2
                    tok0 = T0 + 128 * c
                    k = lambda n: ("op", n, sl)
                    dma("sp", ht[sl], hbuf[tok0:tok0 + 128, :], [("hbuf", tok0)], [k("ht")])
                    tb = 6
                    tpv = resh(psb(tb, 1024), [8, 128])
                    for j in range(8):
                        tr(tpv[:, j, :], yn[sl][:, 128 * j:128 * j + 128], identb, [k("yn"), "identb"], [PK(tb)])
                    cp("act", ynT[sl], tpv, [PK(tb)], [k("ynT")])

                def op_f3(c):
                    sl = c % 2
                    k = lambda n: ("op", n, sl)
                    for nn in range(2):
                        ns = slice(512 * nn, 512 * nn + 512)
                        bnk = 2 * sl + nn
                        for j in range(8):
                            mm(PSB[bnk][:, :], ynT[sl][:, j, :], Wo[:, j, ns], j == 0, j == 7, [k("ynT"), "Wo"], [PK(bnk)])

                def op_back(c):
                    sl = c % 2
                    tok0 = T0 + 128 * c
                    k = lambda n: ("op", n, sl)
                    for nn in range(2):
                        ns = slice(512 * nn, 512 * nn + 512)
                        bnk = 2 * sl + nn
                        stt(acc[sl][:, ns], ht[sl][:, ns], ALPHA, PSB[bnk][:, :], ALU.mult, ALU.add, [k("ht"), PK(bnk)], [k("acc")])
                    return ln_tile(acc[sl], k("acc"), lng, lnb, tok0, c % 3, psbanks=(4, 5), dbgh=(l == 0), staged=True, geng="dve")

                def op_pre(c):
                    if c + 2 < 16:
                        op_f1(c + 2)
                    if c + 1 < 16:
                        op_f2(c + 1)
                    op_f3(c)

                op_f1(0); op_f1(1); op_f2(0)
                ln_pipeline(16, op_back, pre=op_pre)
                A.release("outproj")
                P.barrier()
                A.release()
            A.release()
            P.barrier()

            A.mark()
            LG = A.f32(16, 20)
            A.mark()
            kT = A.bf16(8, 512)
            Vm = A.bf16(4, 1024)
            Wq = A.bf16(8, 1024); Wo2 = A.bf16(8, 1024)
            A.mark()
            Wk = A.bf16(8, 1024); Wv = A.bf16(8, 1024)
            memT = A.bf16(8, 512)
            xm = [A.f32(1024) for _ in range(2)]
            for t in range(2 * SQ, 2 * SQ + 2):
                sl = t % 2
                dma("sp", xm[sl], Din["mem"][128 * t:128 * t + 128, :], (), [("xm", sl)])
                for half in range(2):
                    b = 6 + half
                    for c in range(4):
                        tr(PSB[b][:, 128 * c:128 * c + 128], xm[sl][:, 512 * half + 128 * c:512 * half + 128 * c + 128], identf,
                           [("xm", sl), "identf"], [PK(b)])
                    cp("act" if half == 0 else "dve", memT[:, 4 * half:4 * half + 4, 128 * t:128 * t + 128],
                       resh(psf(b), [4, 128]), [PK(b)], ["memT"])
            dma("pool", Wk, Din["mem_wk"][l].rearrange("(kc p) n -> p kc n", p=128), (), ["Wk"])
            dma("pool", Wv, Din["mem_wv"][l].rearrange("(kc p) n -> p kc n", p=128), (), ["Wv"])
            dma("pool", Wq, Din["mem_wq"][l].rearrange("(kc p) n -> p kc n", p=128), ["memT"], ["Wq"])
            dma("pool", Wo2, Din["mem_wo"][l].rearrange("(kc p) n -> p kc n", p=128), ["memT"], ["Wo2"])
            for fcn in range(8):
                b = fcn % 2
                ms_ = slice(256 * SQ, 256 * SQ + 256)
                for kc in range(8):
                    mm(PSB[b][:, 0:256], Wk[:, kc, 128 * fcn:128 * fcn + 128], memT[:, kc, ms_], kc == 0, kc == 7, ["Wk", "memT"], [PK(b)])
                cp("act" if b == 0 else "dve", kT[:, fcn, ms_], PSB[b][:, 0:256], [PK(b)], ["kT"])
            for t in range(2 * SQ, 2 * SQ + 2):
                for nn in range(2):
                    b = 2 + nn
                    for kc in range(8):
                        mm(PSB[b][:, :], memT[:, kc, 128 * t:128 * t + 128], Wv[:, kc, 512 * nn:512 * nn + 512], kc == 0, kc == 7, ["Wv", "memT"], [PK(b)])
                    cp("act" if nn == 0 else "dve", Vm[:, t, 512 * nn:512 * nn + 512], PSB[b][:, :], [PK(b)], ["Vm"])
            A.release("crossKV")
            P.barrier()
            lng = A.f32(1024); lnb = A.f32(1024)
            ln_tmp = [(A.f32(2, 6), A.f32(2), A.f32(1), A.f32(1), A.f32(1), A.f32(1024), A.f32(1024)) for _ in range(3)]
            ht = [A.f32(1024) for _ in range(2)]
            acc = [A.f32(1024) for _ in range(2)]
            qT2 = [A.bf16(8, 512) for _ in range(2)]
            PTm = [A.bf16(2, 512) for _ in range(2)]
            Rr = [A.f32(512) for _ in range(2)]
            oT2 = [A.bf16(8, 512) for _ in range(2)]
            wr = A.f32(8, 20)
            brb = A.f32(20)
            hTf = [A.f32(8, 128) for _ in range(2)]
            dma("sp", wr[:, :, 0:4], Din["moe_wr_g"][l].rearrange("(kc p) n -> p kc n", p=128), (), ["wr"])
            dma("sp", wr[:, :, 4:20], Din["moe_wr_e"][l].rearrange("(kc p) n -> p kc n", p=128), (), ["wr"])
            dma("sp", brb[:, 0:4], Din["moe_br_g"][l].partition_broadcast(128), (), ["brb"])
            dma("sp", brb[:, 4:20], Din["moe_br_e"][l].partition_broadcast(128), (), ["brb"])
            load_ln("ln2_g", "ln2_b", l)

            def emit_qT(g, fcns):
                tok = slice(512 * (g - 4 * SQ), 512 * (g - 4 * SQ) + 512)
                for fcn in fcns:
                    b = fcn % 2
                    for kc in range(8):
                        mm(PSB[b][:, :], Wq[:, kc, 128 * fcn:128 * fcn + 128], hT[:, kc, tok], kc == 0, kc == 7, ["Wq"], [PK(b)])
                    cp("act" if b == 0 else "dve", qT2[g % 2][:, fcn, :], PSB[b][:, :], [PK(b)], [("qT", g % 2, fcn)])

            def emit_scores(g, h):
                s = g // 4
                qT = qT2[g % 2]
                sl = h % 2
                for kt in range(2):
                    b = 2 + 2 * sl + kt
                    for dc in range(2):
                        mm(PSB[b][:, :], kT[:, 2 * h + dc, 256 * s + 128 * kt:256 * s + 128 * kt + 128], qT[:, 2 * h + dc, :], dc == 0, dc == 1,
                           ["kT", ("qT", g % 2, 2 * h), ("qT", g % 2, 2 * h + 1)], [PK(b)])
                    act(PTm[sl][:, kt, :], PSB[b][:, :], AF.Exp, [PK(b)], [("PTm", sl)], scale=1.0 / 16.0)

            def emit_pv(g, h):
                s = g // 4
                sl = h % 2
                for kt in range(2):
                    mm(PSB[6][:, :], onesb, PTm[sl][:, kt, :], kt == 0, kt == 1, [("PTm", sl), "onesb"], [PK(6)])
                recip(Rr[sl], PSB[6][:, :], [PK(6)], [("Rr", sl)])
                for dc in range(2):
                    b = 7 - dc
                    for kt in range(2):
                        mm(PSB[b][:, :], Vm[:, 2 * s + kt, 256 * h + 128 * dc:256 * h + 128 * dc + 128], PTm[sl][:, kt, :], kt == 0, kt == 1,
                           [("PTm", sl), "Vm"], [PK(b)])
                    tt(oT2[g % 2][:, 2 * h + dc, :], PSB[b][:, :], Rr[sl], ALU.mult, [PK(b), ("Rr", sl)], [("oT", g % 2, 2 * h + dc)])

            def tail_load(c):
                tok0 = 128 * c
                dma("act", ht[c % 2], hbuf[tok0:tok0 + 128, :], [("hbuf", tok0)], [("xo", "ht", c % 2)])

            def tail_mm(g, t4):
                oT_ = oT2[g % 2]
                for nn in range(2):
                    ns = slice(512 * nn, 512 * nn + 512)
                    for kc in range(8):
                        mm(PSB[nn][:, :], oT_[:, kc, 128 * t4:128 * t4 + 128], Wo2[:, kc, ns], kc == 0, kc == 7, [("oT", g % 2, kc), "Wo2"], [PK(nn)])

            def tail_back(g, t4):
                c = 4 * g + t4
                sl = c % 2
                k = lambda n: ("xo", n, sl)
                for nn in range(2):
                    ns = slice(512 * nn, 512 * nn + 512)
                    stt(acc[sl][:, ns], ht[sl][:, ns], ALPHA, PSB[nn][:, :], ALU.mult, ALU.add, [k("ht"), PK(nn)], [k("acc")])
                stg_ = ln_tile(acc[sl], k("acc"), lng, lnb, 128 * c, c % 3, psbanks=(0, 1), staged=True, tcopy=(hTf[sl], ("hTf", sl)), geng="pool", beng="dve")

                def logits():
                    for kc in range(8):
                        mm(PSB[1][:, 0:20], hTf[sl][:, kc, :], wr[:, kc, :], kc == 0, kc == 7, [("hTf", sl), "wr"], [PK(1)])
                    tt(LG[:, c - 16 * SQ, :], PSB[1][:, 0:20], brb, ALU.add, [PK(1), "brb"], [("LG", c)])

                return stg_ + [logits]

            g0 = 4 * SQ
            pstg = []

            def pipe_step(make):
                it = len(pstg)
                pstg.append(make() if make is not None else None)
                if pstg[it] is not None:
                    pstg[it][0]()
                for kk in (1, 2, 4, 3):
                    j = it - kk
                    if j >= 0 and pstg[j] is not None:
                        pstg[j][kk]()

            def tail_step(g, t4):
                c = 4 * g + t4
                if c + 1 < 4 * g0 + 16:
                    tail_load(c + 1)
                tail_mm(g, t4)
                pipe_step(lambda: tail_back(g, t4))

            emit_qT(g0, range(8))
            tail_load(4 * g0)
            for g in range(g0, g0 + 4):
                emit_scores(g, 0)
                for h in range(4):
                    if h + 1 < 4:
                        emit_scores(g, h + 1)
                    if g + 1 < g0 + 4:
                        emit_qT(g + 1, (2 * h, 2 * h + 1))
                    emit_pv(g, h)
                    if g > g0:
                        tail_step(g - 1, h)
            for t4 in range(4):
                tail_step(g0 + 3, t4)
            for _ in range(4):
                pipe_step(None)
            A.release("cross")
            P.barrier()

            A.mark()
            combT = A.bf16(NTOK)
            selE = A.bf16(16, 128)
            Wgu = [A.bf16(8, 512) for _ in range(2)]
            Wd = [A.bf16(2, 1024) for _ in range(2)]

            def load_expert(e, ws):
                dma("pool", Wgu[ws][:, :, 0:256], Din["moe_w_gate"][l][e].rearrange("(kc p) n -> p kc n", p=128), (), [("Wgu", ws)])
                dma("pool", Wgu[ws][:, :, 256:512], Din["moe_w_up"][l][e].rearrange("(kc p) n -> p kc n", p=128), (), [("Wgu", ws)])
                dma("pool", Wd[ws], Din["moe_w_down"][l][e].rearrange("(kc p) n -> p kc n", p=128), (), [("Wd", ws)])

            load_expert(0, 0)
            load_expert(1, 1)
            A.mark()
            NT = 16
            R1 = [A.f32(NT) for _ in range(10)]
            R4 = [A.f32(NT, 4) for _ in range(8)]
            R16 = [A.f32(NT, 16) for _ in range(2)]
            cp("dve", selE[0:16], identf[0:16, 0:16].unsqueeze(2).to_broadcast([16, 16, 128]), ["identf"], ["selE"])
            rk = ["rtb"]
            gl_ = LG[:, :, 0:4]
            el_ = LG[:, :, 4:20].rearrange("p c (g e) -> p c g e", g=4)
            gmax, ngm, gsum, gw, m1_, m2_, dlt, w2_, w1_ = R1[0:9]
            goh, gex, eig, oh1, eig2, oh2, loc, tmp4 = R4
            em, comb = R16
            b3 = lambda x: x.unsqueeze(2).to_broadcast([128, NT, 4])
            P.op("dve", lambda e: e.tensor_reduce(out=gmax, in_=gl_, axis=AX.X, op=ALU.max), ["LG"] + rk, rk)
            tt(goh, gl_, b3(gmax), ALU.is_equal, ["LG"] + rk, rk)
            tt(gex, gl_, b3(gmax), ALU.subtract, ["LG"] + rk, rk)
            act(gex, gex, AF.Exp, rk, rk)
            P.op("dve", lambda e: e.tensor_reduce(out=gsum, in_=gex, axis=AX.X, op=ALU.add), rk, rk)
            recip(gw, gsum, rk, rk)
            em4 = em.rearrange("p c (g e) -> p c g e", g=4)
            tt(em4, el_, goh.unsqueeze(3).to_broadcast([128, NT, 4, 4]), ALU.mult, ["LG"] + rk, rk)
            P.op("dve", lambda e: e.tensor_reduce(out=eig, in_=em.rearrange("p c (g e) -> p c e g", g=4), axis=AX.X, op=ALU.add), rk, rk)
            P.op("dve", lambda e: e.tensor_reduce(out=m1_, in_=eig, axis=AX.X, op=ALU.max), rk, rk)
            tt(oh1, eig, b3(m1_), ALU.is_equal, rk, rk)
            stt(eig2, oh1, -1e30, eig, ALU.mult, ALU.add, rk, rk)
            P.op("dve", lambda e: e.tensor_reduce(out=m2_, in_=eig2, axis=AX.X, op=ALU.max), rk, rk)
            tt(oh2, eig2, b3(m2_), ALU.is_equal, rk, rk)
            tt(dlt, m2_, m1_, ALU.subtract, rk, rk)
            act(w2_, dlt, AF.Sigmoid, rk, rk)
            ts(w1_, w2_, -1.0, 1.0, ALU.mult, ALU.add, rk, rk)
            tt(loc, oh1, b3(w1_), ALU.mult, rk, rk)
            tt(tmp4, oh2, b3(w2_), ALU.mult, rk, rk)
            tt(loc, loc, tmp4, ALU.add, rk, rk)
            tt(loc, loc, b3(gw), ALU.mult, rk, rk)
            comb4 = comb.rearrange("p c (g e) -> p c g e", g=4)
            tt(comb4, goh.unsqueeze(3).to_broadcast([128, NT, 4, 4]), loc.unsqueeze(2).to_broadcast([128, NT, 4, 4]), ALU.mult, rk, rk)
            for c4 in range(4):
                tb = 4 + (c4 % 2)
                for t in range(4):
                    c = 4 * c4 + t
                    tr(PSB[tb][0:16, 128 * t:128 * t + 128], comb[:, c, :], identf, rk + ["identf"], [PK(tb)])
                cp("act", combT[0:16, T0 + 512 * c4:T0 + 512 * c4 + 512], PSB[tb][0:16, :], [PK(tb)], ["combT"])
            A.release("router")
            P.barrier()
            lng = A.f32(1024); lnb = A.f32(1024)
            ln_tmp = [(A.f32(2, 6), A.f32(2), A.f32(1), A.f32(1), A.f32(1), A.f32(1024), A.f32(1024)) for _ in range(3)]
            ht = [A.f32(1024) for _ in range(2)]
            TGS = 1024
            accG2 = [A.f32(TGS // 128, 1024) for _ in range(2)]
            HEp = [A.bf16(2, 2, TGS) for _ in range(2)]
            sgt = [A.f32(512) for _ in range(2)]
            upt = [A.f32(512) for _ in range(2)]
            load_ln("ln3_g", "ln3_b", l)
            drot = 0

            def moe_load(G, t8):
                c = (TGS * G) // 128 + t8
                tok0 = 128 * c
                dma("act", ht[c % 2], hbuf[tok0:tok0 + 128, :], [("hbuf", tok0)], [("mo_ht", c % 2)])

            def moe_tail(G, t8):
                accG = accG2[G % 2]
                c = (TGS * G) // 128 + t8
                sl = c % 2
                tok0 = 128 * c
                stt(accG[:, t8, :], ht[sl], ALPHA, accG[:, t8, :], ALU.mult, ALU.add, [("mo_ht", sl), ("accG", G % 2, t8)], [("accG", G % 2, t8)])
                return ln_tile(accG[:, t8, :], ("accG", G % 2, t8), lng, lnb, tok0, c % 3, final=(l == 1), psbanks=(0, 1), staged=True, geng="dve")

            G0 = 2 * SQ
            for G in range(G0, G0 + 2):
                accG = accG2[G % 2]
                for ep in range(8):
                    hb = ep % 2
                    micro = []
                    if G > G0:
                        if ep == 0:
                            moe_load(G - 1, 0)
                        if ep + 1 < 8:
                            moe_load(G - 1, ep + 1)
                        micro = [lambda G=G, ep=ep: micro.extend(moe_tail(G - 1, ep))]
                    for ei in range(2):
                        e = 2 * ep + ei
                        ws = ei
                        if not (G == G0 and ep == 0):
                            load_expert(e, ws)
                        for q5 in range(TGS // 512):
                            tok = slice(TGS * G + 512 * q5, TGS * G + 512 * q5 + 512)
                            tokl = slice(TGS * G + 512 * q5 - T0, TGS * G + 512 * q5 + 512 - T0)
                            mm(PSB[6][:, :], selE[0:16, e, :], combT[0:16, tok], True, True, ["selE", "combT"], [PK(6)])
                            for dc in range(2):
                                sl = dc
                                for kc in range(8):
                                    mm(PSB[2 * dc][:, :], Wgu[ws][:, kc, 128 * dc:128 * dc + 128], hT[:, kc, tokl], kc == 0, kc == 7, [("Wgu", ws)], [PK(2 * dc)])
                                for kc in range(8):
                                    mm(PSB[2 * dc + 1][:, :], Wgu[ws][:, kc, 256 + 128 * dc:256 + 128 * dc + 128], hT[:, kc, tokl], kc == 0, kc == 7, [("Wgu", ws)], [PK(2 * dc + 1)])
                                act(sgt[sl], PSB[2 * dc][:, :], AF.Silu, [PK(2 * dc)], [("sgt", sl)])
                                tt(upt[sl], PSB[2 * dc + 1][:, :], sgt[sl], ALU.mult, [PK(2 * dc + 1), ("sgt", sl)], [("upt", sl)])
                                tt(HEp[hb][:, ei, dc, 512 * q5:512 * q5 + 512], PSB[6][:, :], upt[sl], ALU.mult, [PK(6), ("upt", sl)], [("HE", hb, ei)])
                            if micro:
                                if ei == 0 and q5 == 0:
                                    micro.pop(0)()
                                    micro.pop(0)()
                                elif len(micro) > 1:
                                    micro.pop(0)()
                    if micro:
                        while len(micro) > 1:
                            micro.pop(0)()
                        pb_ = ((4, 5, 7)[drot % 3], (4, 5, 7)[(drot + 1) % 3])
                        drot += 2
                        micro.pop(0)(pb_)
                    if G == G0 + 1 and ep == 7:
                        moe_load(G0 + 1, 0)
                    for t8 in range(TGS // 128):
                        for nn in range(2):
                            ns = slice(512 * nn, 512 * nn + 512)
                            b = (4, 5, 7)[drot % 3]
                            drot += 1
                            for ei in range(2):
                                for dc in range(2):
                                    mm(PSB[b][:, :], HEp[hb][:, ei, dc, 128 * t8:128 * t8 + 128], Wd[ei][:, dc, ns], ei == 0 and dc == 0, ei == 1 and dc == 1,
                                       [("HE", hb, 0), ("HE", hb, 1), ("Wd", 0), ("Wd", 1)], [PK(b)])
                            if ep == 0:
                                cp("act", accG[:, t8, ns], PSB[b][:, :], [PK(b), ("accG", G % 2, t8)], [("accG", G % 2, t8)])
                            else:
                                tt(accG[:, t8, ns], PSB[b][:, :], accG[:, t8, ns], ALU.add, [PK(b), ("accG", G % 2, t8)], [("accG", G % 2, t8)])

            def drain_pre(i):
                if i + 1 < TGS // 128:
                    moe_load(G0 + 1, i + 1)

            ln_pipeline(TGS // 128, lambda i: moe_tail(G0 + 1, i), pre=drain_pre)
            A.release("moe")
            A.release()
            P.barrier()

    P.emit(st)
    st.close()
    return nc


_CACHE = {}


def kernel(**inputs):
    if "nc" not in _CACHE:
        _CACHE["nc"] = build_program()
    nc = _CACHE["nc"]
    consts = host_consts()
    x = np.ascontiguousarray(inputs["x"], dtype=np.float32)
    mem = np.ascontiguousarray(inputs["mem"], dtype=np.float32)
    in_maps = []
    for c in range(NCORES):
        m = {"x": x[2 * c:2 * c + 2].reshape(NTOK, D), "mem": mem[2 * c:2 * c + 2].reshape(512, D)}
        for k in PARAM_SHAPES:
            m[k] = np.ascontiguousarray(inputs[k], dtype=np.float32)
        for k, v in consts.items():
            m[k] = v
        in_maps.append(m)
    res = run_bass_kernel_spmd(nc, in_maps, core_ids=list(range(NCORES)))
    outs = [np.asarray(r["out"]).reshape(2, S, D) for r in res.results]
    full = np.concatenate(outs, axis=0).astype(np.float32)
    if DEBUG:
        kernel.dbg = [{k: np.asarray(v) for k, v in r.items() if k.startswith("dbg_")} for r in res.results]
    return full
```
